# Optimizing a Trainium2 kernel written in Bass

```python
import jax
import jax.numpy as jnp
from jax import lax
import numpy as np

D_MODEL = 2048
BATCH = 8
SEQ = 2048
DEPTH = 2

CHUNK = 64
EPS = 1e-6
NEG = -1e30
CONV_K = 4

HD_A = 128
W_A = D_MODEL // 2
H_A = W_A // HD_A
LOOKBACK = 8
BAND = (LOOKBACK + 1) * CHUNK
REL_MAX = 256
H_B = 4
W_B = D_MODEL // 2
HD_B = W_B // H_B
FORGET_BIAS = 3.0
W_C = D_MODEL // 2
N_BLK_C = 8
BLK_C = W_C // N_BLK_C
C_RG = 8.0
HD_D = 128
W_D = D_MODEL // 2
H_D = W_D // HD_D

EV_SIZES = (W_A, W_A, W_A, W_A, W_B, W_B, W_B, W_B, W_B, 2 * H_B)
OD_SIZES = (W_C, W_C, W_D, W_D, W_D, W_D, H_D, H_D)
EV_IN = sum(EV_SIZES)
OD_IN = sum(OD_SIZES)
W_EV = W_A + W_B
W_OD = W_C + W_D
N_EVEN = (DEPTH + 1) // 2
N_ODD = DEPTH // 2

kernel_name = "hybrid_chunkattn_mlstm_rglru_gdn"


def split_cols(t, sizes):
    offs = np.cumsum(sizes)[:-1].tolist()
    return jnp.split(t, offs, axis=-1)


def rmsnorm(x, g):
    xf = x.astype(jnp.float32)
    y = xf * lax.rsqrt(jnp.mean(xf * xf, axis=-1, keepdims=True) + EPS)
    return (y * g.astype(jnp.float32)).astype(x.dtype)


def l2norm(x):
    return x * lax.rsqrt(jnp.sum(x * x, axis=-1, keepdims=True) + EPS)


def causal_dwconv(x, w):
    s = x.shape[1]
    xp = jnp.pad(x, ((0, 0), (CONV_K - 1, 0), (0, 0)))
    return sum(xp[:, j:j + s] * w[j] for j in range(CONV_K))


def to_chunks(t):
    b, s, h = t.shape[:3]
    t = t.reshape(b, s // CHUNK, CHUNK, h, *t.shape[3:])
    return jnp.moveaxis(t, (1, 3), (0, 2))


def from_chunks(t):
    nc, b, h, l, d = t.shape
    return jnp.moveaxis(t, (0, 2), (1, 3)).reshape(b, nc * l, h * d)


def chunk_rel_attention(q, k, v, qn_g, kn_g, rel_bias):
    b, s, h, d = q.shape
    nc = s // CHUNK
    q = rmsnorm(q, qn_g).astype(jnp.float32) * (d ** -0.5)
    k = rmsnorm(k, kn_g).astype(jnp.float32)
    v = v.astype(jnp.float32)
    qc = q.reshape(b, nc, CHUNK, h, d)
    pad = ((0, 0), (LOOKBACK, 0), (0, 0), (0, 0), (0, 0))
    kp = jnp.pad(k.reshape(b, nc, CHUNK, h, d), pad)
    vp = jnp.pad(v.reshape(b, nc, CHUNK, h, d), pad)
    idx = jnp.arange(nc)[:, None] + jnp.arange(LOOKBACK + 1)[None, :]
    kb = kp[:, idx].reshape(b, nc, BAND, h, d)
    vb = vp[:, idx].reshape(b, nc, BAND, h, d)
    sc = jnp.einsum('bnqhd,bnkhd->bhnqk', qc, kb)
    qpos = LOOKBACK * CHUNK + jnp.arange(CHUNK)
    kpos = jnp.arange(BAND)
    rel = jnp.clip(qpos[:, None] - kpos[None, :], -REL_MAX, REL_MAX) + REL_MAX
    sc = sc + rel_bias.astype(jnp.float32)[:, rel][None, :, None]
    valid = (idx - LOOKBACK) >= 0
    valid = jnp.repeat(valid, CHUNK, axis=1)
    sc = jnp.where(valid[None, None, :, None, :], sc, NEG)
    p = jax.nn.softmax(sc, axis=-1)
    o = jnp.einsum('bhnqk,bnkhd->bnqhd', p, vb)
    return o.reshape(b, s, h * d)


def mlstm_chunkwise(q, k, v, li, lf):
    b, s, h, d = q.shape
    q = q * (d ** -0.5)
    qc, kc, vc = to_chunks(q), to_chunks(k), to_chunks(v)
    lic, lfc = to_chunks(li), to_chunks(lf)
    tril = jnp.tril(jnp.ones((CHUNK, CHUNK), dtype=bool))

    def step(carry, inp):
        cm, nm, m = carry
        qn, kn, vn, lin, lfn = inp
        bcum = jnp.cumsum(lfn, axis=-1)
        dmat = bcum[..., :, None] - bcum[..., None, :] + lin[..., None, :]
        dmat = jnp.where(tril, dmat, NEG)
        m_t = jnp.maximum(bcum + m[..., None], jnp.max(dmat, axis=-1))
        w_inter = jnp.exp(bcum + m[..., None] - m_t)
        p = jnp.exp(dmat - m_t[..., None]) * jnp.einsum('bhtd,bhsd->bhts', qn, kn)
        num = w_inter[..., None] * jnp.einsum('bhtk,bhkv->bhtv', qn, cm) + jnp.einsum('bhts,bhsv->bhtv', p, vn)
        den = w_inter * jnp.einsum('bhtk,bhk->bht', qn, nm) + jnp.sum(p, axis=-1)
        hout = num / jnp.maximum(jnp.abs(den), jnp.exp(-m_t))[..., None]
        b_last = bcum[..., -1]
        gs = b_last[..., None] - bcum + lin
        m_new = jnp.maximum(b_last + m, jnp.max(gs, axis=-1))
        w_c = jnp.exp(b_last + m - m_new)
        ws = jnp.exp(gs - m_new[..., None])
        cm = w_c[..., None, None] * cm + jnp.einsum('bhs,bhsk,bhsv->bhkv', ws, kn, vn)
        nm = w_c[..., None] * nm + jnp.einsum('bhs,bhsk->bhk', ws, kn)
        return (cm, nm, m_new), hout

    init = (jnp.zeros((b, h, d, d), jnp.float32), jnp.zeros((b, h, d), jnp.float32), jnp.zeros((b, h), jnp.float32))
    _, hs = lax.scan(step, init, (qc, kc, vc, lic, lfc))
    return from_chunks(hs)


def gated_delta_chunkwise(q, k, v, beta, g):
    b, s, h, d = q.shape
    qc = to_chunks(q) * (d ** -0.5)
    kc, vc = to_chunks(k), to_chunks(v)
    bc, gc = to_chunks(beta), to_chunks(g)
    tril = jnp.tril(jnp.ones((CHUNK, CHUNK), dtype=bool))
    strict = jnp.tril(jnp.ones((CHUNK, CHUNK), dtype=bool), k=-1)
    decay = jnp.cumsum(gc, axis=-1)
    gam = jnp.exp(jnp.where(tril, decay[..., :, None] - decay[..., None, :], NEG))
    kbeta = kc * bc[..., None]
    a_mat = jnp.where(strict, jnp.einsum('nbhtd,nbhsd->nbhts', kbeta, kc) * gam, 0.0)
    eye = jnp.eye(CHUNK, dtype=jnp.float32)
    t_inv = lax.linalg.triangular_solve(eye + a_mat, jnp.broadcast_to(eye, a_mat.shape), left_side=True, lower=True, unit_diagonal=True)
    u = jnp.einsum('nbhts,nbhsv->nbhtv', t_inv, vc * bc[..., None])
    w = jnp.einsum('nbhts,nbhsk->nbhtk', t_inv, kbeta * jnp.exp(decay)[..., None])
    qd = qc * jnp.exp(decay)[..., None]
    qk = jnp.einsum('nbhtd,nbhsd->nbhts', qc, kc) * gam
    kd = kc * jnp.exp(decay[..., -1:] - decay)[..., None]
    d_last = jnp.exp(decay[..., -1])

    def step(st, inp):
        un, wn, qdn, qkn, kdn, dln = inp
        v_new = un - jnp.einsum('bhtk,bhkv->bhtv', wn, st)
        o = jnp.einsum('bhtk,bhkv->bhtv', qdn, st) + jnp.einsum('bhts,bhsv->bhtv', qkn, v_new)
        st = dln[..., None, None] * st + jnp.einsum('bhsk,bhsv->bhkv', kdn, v_new)
        return st, o

    init = jnp.zeros((b, h, d, d), jnp.float32)
    _, os_ = lax.scan(step, init, (u, w, qd, qk, kd, d_last))
    return jnp.moveaxis(os_, (0, 2), (1, 3)).reshape(b, s, h, d)


def rg_lru(xconv, gate_w, gate_b, lam):
    b, s, _ = xconv.shape
    gates = jnp.einsum('bsnc,ncd->bsnd', xconv.reshape(b, s, N_BLK_C, BLK_C), gate_w.astype(jnp.float32))
    gb = gate_b.astype(jnp.float32)
    r = jax.nn.sigmoid(gates[..., :BLK_C].reshape(b, s, W_C) + gb[:W_C])
    i = jax.nn.sigmoid(gates[..., BLK_C:].reshape(b, s, W_C) + gb[W_C:])
    log_a = -C_RG * r * jax.nn.softplus(-lam.astype(jnp.float32))
    a = jnp.exp(log_a)
    inp = jnp.sqrt(-jnp.expm1(2.0 * log_a)) * (i * xconv)

    def combine(e1, e2):
        a1, b1 = e1
        a2, b2 = e2
        return a1 * a2, a2 * b1 + b2

    _, hs = lax.associative_scan(combine, (a, inp), axis=1)
    return hs


def even_layer(x, norm_g, w_in, if_bias, qn_g, kn_g, rel_bias, w_out):
    b, s, _ = x.shape
    proj = rmsnorm(x, norm_g) @ w_in
    qa, ka, va, za, qb, kb, vb, ob, zb, gif = split_cols(proj, EV_SIZES)
    ya = chunk_rel_attention(qa.reshape(b, s, H_A, HD_A), ka.reshape(b, s, H_A, HD_A), va.reshape(b, s, H_A, HD_A), qn_g, kn_g, rel_bias)
    ya = ya * jax.nn.silu(za.astype(jnp.float32))
    gif = gif.astype(jnp.float32) + if_bias.astype(jnp.float32)
    li = gif[..., :H_B]
    lf = jax.nn.log_sigmoid(gif[..., H_B:])
    f32 = jnp.float32
    hb = mlstm_chunkwise(qb.astype(f32).reshape(b, s, H_B, HD_B), kb.astype(f32).reshape(b, s, H_B, HD_B), vb.astype(f32).reshape(b, s, H_B, HD_B), li, lf)
    yb = jax.nn.sigmoid(ob.astype(f32)) * hb * jax.nn.silu(zb.astype(f32))
    y = jnp.concatenate([ya, yb], axis=-1).astype(x.dtype)
    return y @ w_out


def odd_layer(x, norm_g, w_in, conv_c_w, conv_c_b, gate_w, gate_b, lam, conv_d_w, a_log, dt_bias, onorm_g, w_out):
    b, s, _ = x.shape
    f32 = jnp.float32
    proj = rmsnorm(x, norm_g) @ w_in
    xc, zc, qd, kd, vd, zd, a_pre, b_pre = split_cols(proj, OD_SIZES)
    xconv = causal_dwconv(xc.astype(f32), conv_c_w.astype(f32)) + conv_c_b.astype(f32)
    yc = rg_lru(xconv, gate_w, gate_b, lam) * jax.nn.silu(zc.astype(f32))
    qkv = jax.nn.silu(causal_dwconv(jnp.concatenate([qd, kd, vd], axis=-1).astype(f32), conv_d_w.astype(f32)))
    q, k, v = jnp.split(qkv, 3, axis=-1)
    q = l2norm(q.reshape(b, s, H_D, HD_D))
    k = l2norm(k.reshape(b, s, H_D, HD_D))
    v = v.reshape(b, s, H_D, HD_D)
    beta = jax.nn.sigmoid(b_pre.astype(f32))
    g = -jnp.exp(a_log.astype(f32)) * jax.nn.softplus(a_pre.astype(f32) + dt_bias.astype(f32))
    od = gated_delta_chunkwise(q, k, v, beta, g)
    yd = rmsnorm(od, onorm_g).reshape(b, s, W_D) * jax.nn.silu(zd.astype(f32))
    y = jnp.concatenate([yc, yd], axis=-1).astype(x.dtype)
    return y @ w_out


def setup_inputs(seed: int = 0) -> dict:
    key = jax.random.key(seed)
    ks = jax.random.split(key, 24)
    f32 = jnp.float32
    ne, no = N_EVEN, N_ODD

    def nrm(k, shape, scale):
        return scale * jax.random.normal(k, shape, f32)

    x = nrm(ks[0], (BATCH, SEQ, D_MODEL), 1.0)
    ev_norm = 1.0 + nrm(ks[1], (ne, D_MODEL), 0.05)
    ev_w_in = nrm(ks[2], (ne, D_MODEL, EV_IN), D_MODEL ** -0.5)
    ev_if_bias = jnp.concatenate([nrm(ks[3], (ne, H_B), 0.1), FORGET_BIAS + nrm(ks[4], (ne, H_B), 0.5)], axis=-1)
    ev_qn_gain = 1.0 + nrm(ks[5], (ne, HD_A), 0.05)
    ev_kn_gain = 1.0 + nrm(ks[6], (ne, HD_A), 0.05)
    ev_rel_bias = nrm(ks[7], (ne, H_A, 2 * REL_MAX + 1), 0.2)
    ev_w_out = nrm(ks[8], (ne, W_EV, D_MODEL), W_EV ** -0.5)
    od_norm = 1.0 + nrm(ks[9], (no, D_MODEL), 0.05)
    od_w_in = nrm(ks[10], (no, D_MODEL, OD_IN), D_MODEL ** -0.5)
    od_conv_c_w = nrm(ks[11], (no, CONV_K, W_C), CONV_K ** -0.5)
    od_conv_c_b = nrm(ks[12], (no, W_C), 0.01)
    od_gate_w = nrm(ks[13], (no, N_BLK_C, BLK_C, 2 * BLK_C), BLK_C ** -0.5)
    od_gate_b = nrm(ks[14], (no, 2 * W_C), 0.1)
    a0 = jax.random.uniform(ks[15], (no, W_C), f32, minval=0.9, maxval=0.999)
    sig = a0 ** (1.0 / C_RG)
    od_lambda = jnp.log(sig) - jnp.log1p(-sig)
    od_conv_d_w = nrm(ks[16], (no, CONV_K, 3 * W_D), CONV_K ** -0.5)
    od_a_log = jnp.log(jax.random.uniform(ks[17], (no, H_D), f32, minval=1.0, maxval=16.0))
    u = jax.random.uniform(ks[18], (no, H_D), f32)
    dt = jnp.exp(np.log(0.001) + u * (np.log(0.1) - np.log(0.001)))
    od_dt_bias = dt + jnp.log(-jnp.expm1(-dt))
    od_onorm = 1.0 + nrm(ks[19], (no, HD_D), 0.05)
    od_w_out = nrm(ks[20], (no, W_OD, D_MODEL), W_OD ** -0.5)
    return {"x": x, "ev_norm": ev_norm, "ev_w_in": ev_w_in, "ev_if_bias": ev_if_bias, "ev_qn_gain": ev_qn_gain, "ev_kn_gain": ev_kn_gain, "ev_rel_bias": ev_rel_bias, "ev_w_out": ev_w_out, "od_norm": od_norm, "od_w_in": od_w_in, "od_conv_c_w": od_conv_c_w, "od_conv_c_b": od_conv_c_b, "od_gate_w": od_gate_w, "od_gate_b": od_gate_b, "od_lambda": od_lambda, "od_conv_d_w": od_conv_d_w, "od_a_log": od_a_log, "od_dt_bias": od_dt_bias, "od_onorm": od_onorm, "od_w_out": od_w_out}


def reference(x, ev_norm, ev_w_in, ev_if_bias, ev_qn_gain, ev_kn_gain, ev_rel_bias, ev_w_out, od_norm, od_w_in, od_conv_c_w, od_conv_c_b, od_gate_w, od_gate_b, od_lambda, od_conv_d_w, od_a_log, od_dt_bias, od_onorm, od_w_out):
    for layer in range(DEPTH):
        j = layer // 2
        if layer % 2 == 0:
            x = x + even_layer(x, ev_norm[j], ev_w_in[j], ev_if_bias[j], ev_qn_gain[j], ev_kn_gain[j], ev_rel_bias[j], ev_w_out[j])
        else:
            x = x + odd_layer(x, od_norm[j], od_w_in[j], od_conv_c_w[j], od_conv_c_b[j], od_gate_w[j], od_gate_b[j], od_lambda[j], od_conv_d_w[j], od_a_log[j], od_dt_bias[j], od_onorm[j], od_w_out[j])
    return x
```

```python
import numpy as np
from contextlib import ExitStack
import concourse.bass as bass
import concourse.mybir as mybir
from concourse.bass_utils import run_bass_kernel_spmd

F32 = mybir.dt.float32
BF16 = mybir.dt.bfloat16
AF = mybir.ActivationFunctionType
ALU = mybir.AluOpType
AX = mybir.AxisListType

S_LEN = 2048
D = 2048
NT = 16
EPS = 1e-6
NEG = -1e30


class Buf:
    __slots__ = ("name", "w", "r")

    def __init__(self, name=""):
        self.name = name
        self.w = None
        self.r = {}


class _FakeInst:
    def then_inc(self, *a, **k):
        return self


class _FakeEng:
    def __init__(self):
        self.calls = []

    def __getattr__(self, name):
        def f(*a, **k):
            self.calls.append((name, a, k))
            return _FakeInst()
        return f


def _free_elems(ap):
    sh = ap.shape
    n = 1
    for d in sh[1:]:
        n *= int(d)
    return n


def _est_cost(eng, fns):
    fk = _FakeEng()
    for f in fns:
        f(fk)
    tot = 0.0
    for (name, a, k) in fk.calls:
        if name == "matmul":
            rhs = k.get("rhs")
            n = _free_elems(rhs)
            mult = 4.0 if rhs.dtype == F32 else 1.0
            tot += mult * max(64, n) * 0.45 + 15
        elif name == "transpose":
            tot += 128 * 0.45 + 30
        else:
            out = k.get("out", a[0] if a else None)
            n = _free_elems(out) if out is not None else 64
            if name == "tensor_tensor_scan":
                n *= 2
            if eng == "pool":
                tot += 350 + 2.0 * n
            elif eng == "act":
                tot += 220 + 1.05 * n
            else:
                tot += 200 + 1.05 * n
    return tot


class Sched:
    ENG = ("pe", "act", "dve", "pool", "sp")
    DMA_POOLS = {"sp": 10, "act": 4, "pool": 8, "pe": 1, "dve": 1}

    def __init__(self, nc, same_engine_wait=True, reorder=True):
        self.nc = nc
        self.same_engine_wait = same_engine_wait
        self.reorder = reorder
        self.nodes = []
        self.segments = []
        self.pending = {e: None for e in self.ENG}
        self.sems = {}
        self.n_ops = 0
        self.n_waits = 0
        self.q = {e: [] for e in self.ENG}
        self.dma_keys = {}
        idx = 0
        for e, n in self.DMA_POOLS.items():
            self.dma_keys[e] = [("dma", idx + t) for t in range(n)]
            idx += n
        self.N_DMA_SEM = idx

    def op(self, eng, fn, reads=(), writes=(), inc=True):
        self.n_ops += 1
        p = self.pending[eng]
        if p is None:
            p = {"eng": eng, "kind": "op", "fns": [], "reads": [], "writes": []}
        p["fns"].append(fn)
        p["reads"].extend(reads)
        p["writes"].extend(writes)
        if inc:
            self.pending[eng] = None
            self.nodes.append(p)
        else:
            self.pending[eng] = p

    def dma(self, eng, out_ap, in_ap, reads=(), writes=(), **kw):
        assert self.pending[eng] is None
        self.n_ops += 1
        self.nodes.append({"eng": eng, "kind": "dma", "out": out_ap, "in": in_ap, "kw": kw, "reads": list(reads), "writes": list(writes)})

    def barrier(self):
        for e in self.ENG:
            assert self.pending[e] is None, e
        self.segments.append(self.nodes)
        self.nodes = []

    def _schedule(self, nodes):
        n = len(nodes)
        preds = [set() for _ in range(n)]
        lastw = {}
        readers = {}
        for i, nd in enumerate(nodes):
            for b in nd["reads"]:
                w = lastw.get(id(b))
                if w is not None:
                    preds[i].add(w)
            for b in nd["writes"]:
                w = lastw.get(id(b))
                if w is not None:
                    preds[i].add(w)
                for r in readers.get(id(b), ()):
                    preds[i].add(r)
            for b in nd["writes"]:
                lastw[id(b)] = i
                readers[id(b)] = []
            for b in nd["reads"]:
                readers.setdefault(id(b), []).append(i)
            preds[i].discard(i)
        if not self.reorder:
            order = {e: [i for i in range(n) if nodes[i]["eng"] == e] for e in self.ENG}
            return preds, order
        cost = []
        for nd in nodes:
            if nd["kind"] == "dma":
                o = nd["out"]
                nbytes = _free_elems(o) * int(o.shape[0]) * (2 if o.dtype == BF16 else 4)
                nd["lat"] = 2200.0 + nbytes / 120.0
                cost.append(900.0 if nd["eng"] == "pool" else 120.0)
            else:
                cost.append(_est_cost(nd["eng"], nd["fns"]))
        succs = [[] for _ in range(n)]
        npred = [len(p) for p in preds]
        for i, p in enumerate(preds):
            for j in p:
                succs[j].append(i)
        done_t = [0.0] * n
        ready_t = [0.0] * n
        avail = {e: [] for e in self.ENG}
        for i in range(n):
            if npred[i] == 0:
                avail[nodes[i]["eng"]].append(i)
        free = {e: 0.0 for e in self.ENG}
        order = {e: [] for e in self.ENG}
        left = n
        WIN = 4000
        low = {e: 0 for e in self.ENG}
        while left:
            best = None
            for e in self.ENG:
                av = avail[e]
                if not av:
                    continue
                fe = free[e]
                m = min(av)
                bi = None
                bk = None
                for i in av:
                    if i > m + WIN:
                        continue
                    key = (max(ready_t[i], fe), i)
                    if bk is None or key < bk:
                        bk = key
                        bi = i
                if best is None or bk < best[0]:
                    best = (bk, e, bi)
            (st, _), e, i = best
            avail[e].remove(i)
            order[e].append(i)
            end = st + cost[i]
            free[e] = end
            done_t[i] = end if nodes[i]["kind"] != "dma" else st + nodes[i]["lat"]
            left -= 1
            for sidx in succs[i]:
                npred[sidx] -= 1
                if done_t[i] > ready_t[sidx]:
                    ready_t[sidx] = done_t[i]
                if npred[sidx] == 0:
                    avail[nodes[sidx]["eng"]].append(sidx)
        return preds, order

    def emit(self, block):
        if self.nodes:
            self.barrier()
        sems = self.sems
        cnt = {e: 0 for e in self.ENG}
        dma_tot = {}
        dma_rr = {e: 0 for e in self.ENG}
        seen = {e: {} for e in self.ENG}
        q = self.q

        def need(eng, evs):
            sn = seen[eng]
            best = {}
            for (k, v) in evs:
                if k == eng and (eng == "pe" or not self.same_engine_wait):
                    continue
                if sn.get(k, 0) >= v:
                    continue
                if best.get(k, 0) < v:
                    best[k] = v
            for k, v in best.items():
                sn[k] = v
                q[eng].append(("w", k, v))
                self.n_waits += 1

        for nodes in self.segments:
            preds, order = self._schedule(nodes)
            ev = [None] * len(nodes)
            for e in self.ENG:
                for i in order[e]:
                    nd = nodes[i]
                    if nd["kind"] == "op":
                        cnt[e] += 1
                        ev[i] = (e, cnt[e])
                    else:
                        keys = self.dma_keys[e]
                        key = keys[dma_rr[e] % len(keys)]
                        dma_rr[e] += 1
                        prev = dma_tot.get(key, 0)
                        nd["prev"] = (key, prev) if prev else None
                        dma_tot[key] = prev + 16
                        nd["key"] = key
                        ev[i] = (key, prev + 16)
            for e in self.ENG:
                for i in order[e]:
                    nd = nodes[i]
                    evs = [ev[j] for j in preds[i]]
                    if nd["kind"] == "dma" and nd["prev"] is not None:
                        evs.append(nd["prev"])
                    need(e, evs)
                    if nd["kind"] == "op":
                        q[e].append(("g", nd["fns"]))
                    else:
                        q[e].append(("d", nd["out"], nd["in"], nd["key"], nd["kw"]))
            allev = [(e, cnt[e]) for e in self.ENG if cnt[e] > 0] + [(k, t) for k, t in dma_tot.items()]
            for e in self.ENG:
                need(e, allev)

        def run(eng_name, e):
            for it in q[eng_name]:
                t = it[0]
                if t == "w":
                    e.wait_ge(sems[it[1]], it[2])
                elif t == "g":
                    fns = it[1]
                    for f in fns[:-1]:
                        f(e)
                    fns[-1](e).then_inc(sems[eng_name], 1)
                elif t == "d":
                    e.dma_start(out=it[1], in_=it[2], **it[4]).then_inc(sems[it[3]], 16)

        @block.tensor
        def _(e):
            run("pe", e)

        @block.scalar
        def _(e):
            run("act", e)

        @block.vector
        def _(e):
            run("dve", e)

        @block.gpsimd
        def _(e):
            run("pool", e)

        @block.sync
        def _(e):
            run("sp", e)


PP_EVN = 0
PP_ODN = 16
PP_QN = 32
PP_KN = 33
PP_ON = 34
PP_CCW = 35
PP_CCB = 67
PP_GBR = 75
PP_GBI = 83
PP_LAM = 91
PP_CDW = 99
PP_N = 195
SBUF_BASE = 16544
NS_MAX = 1000
SBUF_BYTES = 212832


class Ctx:
    pass


class Arena:
    def __init__(self, c, segs=None):
        self.c = c
        self.segs = [list(x) for x in (segs or [(0, 65536), (c.ARENA, SBUF_BYTES)])]

    def __call__(self, shape, dt, nbytes):
        n = _r32(nbytes)
        for sg in self.segs:
            if sg[0] + n <= sg[1]:
                t = self.c.sbt(shape, dt, sg[0])
                sg[0] += n
                return t
        raise AssertionError("SBUF arena overflow %s %d %s" % (shape, nbytes, self.segs))

    def at(self, shape, dt, nbytes):
        n = _r32(nbytes)
        for sg in self.segs:
            if sg[0] + n <= sg[1]:
                off = sg[0]
                t = self.c.sbt(shape, dt, off)
                sg[0] += n
                return t, off
        raise AssertionError("SBUF arena overflow %s %d %s" % (shape, nbytes, self.segs))


def _r32(n):
    return (int(n) + 31) // 32 * 32


def build(phases=("all",), dbg=False):
    nc = bass.Bass("TRN2", target_bir_lowering=False)
    allp = "all" in phases

    def on(p):
        return allp or p in phases

    def dram(name, shape, dt, kind=None):
        if kind is None:
            return nc.dram_tensor(name, shape, dt).ap()
        return nc.dram_tensor(name, shape, dt, kind=kind).ap()

    c = Ctx()
    c.nc = nc
    c.x = dram("x", [S_LEN, D], F32, "ExternalInput")
    c.win = [dram("w_in0", [72, 128, 16, 128], F32, "ExternalInput"), dram("w_in1", [48, 128, 16, 128], F32, "ExternalInput")]
    c.wtail = [dram("w_tail0", [128, 16, 8], F32, "ExternalInput"), dram("w_tail1", [128, 16, 16], F32, "ExternalInput")]
    c.wout = [dram("w_out0", [4, 128, 16, 512], F32, "ExternalInput"), dram("w_out1", [4, 128, 16, 512], F32, "ExternalInput")]
    c.gatew = dram("gate_w", [8, 128, 256], F32, "ExternalInput")
    c.pp = dram("pp", [128, PP_N], F32, "ExternalInput")
    c.hp = dram("hp", [8, 4], F32, "ExternalInput")
    c.bt = dram("bt", [8, 128, 640], F32, "ExternalInput")
    c.out = dram("out", [S_LEN, D], F32, "ExternalOutput")
    sk = lambda name, prod: ("ExternalOutput" if on(prod) else "ExternalInput") if dbg else None
    c.projT = [dram("projT0", [72, 128, S_LEN], BF16, sk("projT0", "gemm0")), dram("projT1", [48, 128, S_LEN], BF16, sk("projT1", "gemm1"))]
    c.ptail = [dram("ptail0", [8, S_LEN], F32, sk("ptail0", "gemm0")), dram("ptail1", [16, S_LEN], F32, sk("ptail1", "gemm1"))]
    c.ydscr = dram("ydscr", [8, 128, S_LEN], BF16)
    c.ydb = [Buf() for _ in range(8)]
    if dbg:
        c.ytd = dram("ytd", [16, 128, S_LEN], BF16, "ExternalOutput")
        c.xtd = dram("xtd", [16, 128, S_LEN], BF16, "ExternalOutput")
        c.ytin = dram("ytin", [16, 128, S_LEN], BF16, "ExternalInput")
        c.x1in = dram("x1in", [S_LEN, D], F32, "ExternalInput")
        c.dbgo = dram("dbgo", [10, 128, S_LEN], F32, "ExternalOutput")
    c.dbg = dbg

    with ExitStack() as es:
        S = Sched(nc)
        c.S = S
        sems = {}
        for e in S.ENG:
            sems[e] = es.enter_context(nc.semaphore("s_" + e))
        for i in range(S.N_DMA_SEM):
            sems[("dma", i)] = es.enter_context(nc.semaphore("d%d" % i))
        S.sems = sems
        c.uid = 0

        def sbt(shape, dt, off, name=None):
            c.uid += 1
            assert off % 32 == 0 and off + 1 <= SBUF_BYTES, off
            return nc.alloc_sbuf_tensor_at(name or ("t%d" % c.uid), shape, dt, offset=SBUF_BASE + off)
        c.sbt = sbt
        c.ps = [es.enter_context(nc.psum_tensor("ps%d" % i, [128, 512], F32)) for i in range(8)]
        c.psb = [Buf("ps%d" % i) for i in range(8)]
        block = es.enter_context(nc.Block())

        c.XT = sbt([128, 16, S_LEN], BF16, 0, "XT")
        c.YT = sbt([128, 16, S_LEN], BF16, 65536, "YT")
        c.xtb = [Buf("xt%d" % i) for i in range(16)]
        c.ytb = [Buf("yt%d" % i) for i in range(16)]
        CO = 131072
        c.ident = sbt([128, 128], BF16, CO, "ident"); CO += _r32(256)
        c.identf = sbt([128, 128], F32, CO, "identf"); CO += _r32(512)
        c.ones = sbt([128, 128], BF16, CO, "ones"); CO += _r32(256)
        c.ppt = sbt([128, PP_N], F32, CO, "ppt"); CO += _r32(PP_N * 4)
        c.hpt = sbt([8, 4], F32, CO, "hpt"); CO += _r32(16)
        c.sel = sbt([40, 8, 128], F32, CO, "sel"); CO += _r32(8 * 128 * 4)
        c.m_iu = sbt([128, 128], F32, CO, "m_iu"); CO += _r32(512)
        c.nmu_d = sbt([128, 128], F32, CO, "nmu_d"); CO += _r32(512)
        c.nml_d = sbt([128, 128], F32, CO, "nml_d"); CO += _r32(512)
        c.ml_1 = sbt([128, 128], F32, CO, "ml_1"); CO += _r32(512)
        c.ml_2 = sbt([128, 128], F32, CO, "ml_2"); CO += _r32(512)
        c.cmask = sbt([8, S_LEN], BF16, CO, "cmask"); CO += _r32(S_LEN * 2)
        c.negu2 = sbt([128, 2, 128], F32, CO, "negu2"); CO += _r32(1024)
        c.posl3 = sbt([128, 3, 128], F32, CO, "posl3"); CO += _r32(1536)
        c.epst = sbt([128, 4], F32, CO, "epst"); CO += _r32(16)
        c.cb = Buf("consts")
        c.ARENA = CO
        assert CO <= 131072 + 15872, CO
        c.ARENA = 131072 + 15872

        setup_consts(c)
        S.barrier()
        for L in (0, 1):
            if on("norm%d" % L):
                src = c.x if L == 0 else (c.x1in if (dbg and not on("outp0")) else c.out)
                phase_norm(c, L, src)
                S.barrier()
                if dbg:
                    dump_T(c, c.XT, c.xtd)
                    S.barrier()
            if on("gemm%d" % L):
                phase_gemm(c, L)
                S.barrier()
            if L == 0:
                if on("mixa"):
                    phase_mix_a(c)
                    S.barrier()
                if on("mixb"):
                    phase_mix_b(c)
                    S.barrier()
            else:
                if on("mixc"):
                    phase_mix_c(c)
                    S.barrier()
                if on("mixd"):
                    phase_mix_d(c)
                    S.barrier()
                    for h_ in range(8):
                        S.dma("sp", c.YT[:, 8 + h_, :], c.ydscr[h_], reads=[c.ydb[h_]], writes=[c.ytb[8 + h_]])
                    S.barrier()
            if dbg and (on("mixa") or on("mixb") or on("mixc") or on("mixd")):
                dump_T(c, c.YT, c.ytd)
                S.barrier()
            if on("outp%d" % L):
                if dbg and not (on("mixa") or on("mixb") or on("mixc") or on("mixd")):
                    load_T(c, c.YT, c.ytin)
                    S.barrier()
                src = c.x if L == 0 else (c.x1in if (dbg and not on("outp0")) else c.out)
                phase_outp(c, L, src)
                S.barrier()
        S.emit(block)
    c.nc = nc
    return nc, S


def dump_T(c, T, dst):
    S = c.S
    for s in range(16):
        ev = S.dma("sp", dst[s], T[:, s, :])
    S.barrier()


def load_T(c, T, src):
    S = c.S
    for s in range(16):
        S.dma("sp", T[:, s, :], src[s])


def setup_consts(c):
    S = c.S
    cb = c.cb
    W = [cb]
    S.dma("sp", c.ppt[:], c.pp, writes=W)
    S.dma("sp", c.hpt[:], c.hp, writes=W)
    P = lambda fn: S.op("pool", fn, reads=W, writes=W)
    P(lambda e: e.memset(c.ident[:], 0.0))
    P(lambda e: e.affine_select(out=c.ident[:], in_=c.ident[:], pattern=[[-1, 128]], compare_op=ALU.not_equal, fill=1.0, base=0, channel_multiplier=1))
    P(lambda e: e.memset(c.identf[:], 0.0))
    P(lambda e: e.affine_select(out=c.identf[:], in_=c.identf[:], pattern=[[-1, 128]], compare_op=ALU.not_equal, fill=1.0, base=0, channel_multiplier=1))
    P(lambda e: e.memset(c.ones[:], 1.0))
    P(lambda e: e.memset(c.epst[:, 0:1], EPS))
    P(lambda e: e.memset(c.epst[:, 1:2], 128 * EPS))
    P(lambda e: e.memset(c.epst[:, 2:3], 1.0))
    P(lambda e: e.memset(c.epst[:, 3:4], 0.0))
    P(lambda e: e.memset(c.sel[:], 0.0))
    P(lambda e: e.affine_select(out=c.sel[:], in_=c.sel[:], pattern=[[-1, 8], [0, 128]], compare_op=ALU.not_equal, fill=1.0, base=0, channel_multiplier=1))
    P(lambda e: e.affine_select(out=c.sel[:], in_=c.sel[:], pattern=[[-1, 8], [0, 128]], compare_op=ALU.not_equal, fill=1.0, base=-32, channel_multiplier=1))
    P(lambda e: e.memset(c.cmask[:], 1.0))
    P(lambda e: e.memset(c.cmask[:].rearrange("p (j t) -> p j t", t=128)[:, :, 0:1], 0.0))
    P(lambda e: e.memset(c.m_iu[:], 1.0))
    P(lambda e: e.affine_select(out=c.m_iu[:], in_=c.m_iu[:], pattern=[[1, 128]], compare_op=ALU.is_ge, fill=0.0, base=0, channel_multiplier=-1))
    A = c.ARENA
    su = c.sbt([128, 128], F32, A)
    sl = c.sbt([128, 128], F32, A + 512)
    b32 = c.sbt([128, 128], F32, A + 1024)
    b64 = c.sbt([128, 128], F32, A + 1536)
    e32 = c.sbt([4, 128], F32, A + 2048)
    e64 = c.sbt([2, 128], F32, A + 2560)
    tmp = c.sbt([128, 128], F32, A + 3072)
    P(lambda e: e.memset(su[:], 1.0))
    P(lambda e: e.affine_select(out=su[:], in_=su[:], pattern=[[1, 128]], compare_op=ALU.is_gt, fill=0.0, base=0, channel_multiplier=-1))
    P(lambda e: e.memset(sl[:], 1.0))
    P(lambda e: e.affine_select(out=sl[:], in_=sl[:], pattern=[[-1, 128]], compare_op=ALU.is_gt, fill=0.0, base=0, channel_multiplier=1))
    for (et, bs) in ((e32, 32), (e64, 64)):
        P(lambda e, et=et: e.memset(et[:], 1.0))
        P(lambda e, et=et, bs=bs: e.affine_select(out=et[:], in_=et[:], pattern=[[1, 128]], compare_op=ALU.is_ge, fill=0.0, base=0, channel_multiplier=-bs))
        P(lambda e, et=et, bs=bs: e.affine_select(out=et[:], in_=et[:], pattern=[[-1, 128]], compare_op=ALU.is_ge, fill=0.0, base=bs - 1, channel_multiplier=bs))
    pb = c.psb[0]
    S.op("pe", lambda e: e.matmul(c.ps[0][:, 0:128], lhsT=e32[:], rhs=e32[:], start=True, stop=True), reads=W, writes=[pb])
    S.op("dve", lambda e: e.tensor_copy(out=b32[:], in_=c.ps[0][:, 0:128]), reads=[pb], writes=W)
    S.op("pe", lambda e: e.matmul(c.ps[0][:, 128:256], lhsT=e64[:], rhs=e64[:], start=True, stop=True), reads=W, writes=[pb])
    S.op("dve", lambda e: e.tensor_copy(out=b64[:], in_=c.ps[0][:, 128:256]), reads=[pb], writes=W)
    V = lambda fn: S.op("dve", fn, reads=W, writes=W)
    V(lambda e: e.scalar_tensor_tensor(out=c.nmu_d[:], in0=su[:], scalar=-1.0, in1=b32[:], op0=ALU.mult, op1=ALU.mult))
    V(lambda e: e.scalar_tensor_tensor(out=c.nml_d[:], in0=sl[:], scalar=-1.0, in1=b32[:], op0=ALU.mult, op1=ALU.mult))
    V(lambda e: e.tensor_tensor(out=tmp[:], in0=b64[:], in1=b32[:], op=ALU.subtract))
    V(lambda e: e.tensor_tensor(out=c.ml_1[:], in0=sl[:], in1=tmp[:], op=ALU.mult))
    V(lambda e: e.tensor_scalar(out=tmp[:], in0=b64[:], scalar1=-1.0, scalar2=1.0, op0=ALU.mult, op1=ALU.add))
    V(lambda e: e.tensor_tensor(out=c.ml_2[:], in0=sl[:], in1=tmp[:], op=ALU.mult))
    BIG = 1.0e4
    V(lambda e: e.tensor_scalar(out=c.negu2[:, 0, :], in0=c.m_iu[:], scalar1=BIG, scalar2=-BIG, op0=ALU.mult, op1=ALU.add))
    V(lambda e: e.tensor_scalar(out=c.negu2[:, 1, :], in0=c.nmu_d[:], scalar1=-BIG, scalar2=-BIG, op0=ALU.mult, op1=ALU.add))
    V(lambda e: e.tensor_scalar(out=c.posl3[:, 0, :], in0=c.nml_d[:], scalar1=BIG, scalar2=BIG, op0=ALU.mult, op1=ALU.add))
    V(lambda e: e.tensor_scalar(out=c.posl3[:, 1, :], in0=c.ml_1[:], scalar1=-BIG, scalar2=BIG, op0=ALU.mult, op1=ALU.add))
    V(lambda e: e.tensor_scalar(out=c.posl3[:, 2, :], in0=c.ml_2[:], scalar1=-BIG, scalar2=BIG, op0=ALU.mult, op1=ALU.add))


def phase_norm(c, L, src):
    S = c.S
    A = c.ARENA
    xin = [c.sbt([128, D], F32, A + i * 8192) for i in range(3)]
    xinb = [Buf() for _ in range(3)]
    xs = [c.sbt([128, D], BF16, A + 24576 + i * 4096) for i in range(2)]
    xsb = [Buf() for _ in range(2)]
    junk = c.sbt([128, D], BF16, A + 32768)
    junkb = Buf()
    st = c.sbt([128, 64], F32, A + 36864)
    stb = [Buf() for _ in range(2)]
    goff = PP_EVN if L == 0 else PP_ODN
    for tt in range(NT):
        i = tt % 2
        i3 = tt % 3
        S.dma("sp", xin[i3][:], src[tt * 128:(tt + 1) * 128, :], writes=[xinb[i3]])
        ss = st[:, 2 * i:2 * i + 1]
        rs = st[:, 2 * i + 1:2 * i + 2]
        S.op("act", lambda e, i3=i3, ss=ss: e.activation(out=junk[:], in_=xin[i3][:], func=AF.Square, accum_out=ss), reads=[xinb[i3]], writes=[junkb, stb[i]])
        S.op("act", lambda e, ss=ss, rs=rs: e.activation(out=rs, in_=ss, func=AF.Sqrt, scale=1.0 / D, bias=c.epst[:, 0:1]), reads=[stb[i], c.cb], writes=[stb[i]])
        S.op("dve", lambda e, rs=rs: e.reciprocal(out=rs, in_=rs), reads=[stb[i]], writes=[stb[i]])
        S.op("act", lambda e, i=i, i3=i3, rs=rs: e.activation(out=xs[i][:], in_=xin[i3][:], func=AF.Copy, scale=rs), reads=[xinb[i3], stb[i]], writes=[xsb[i]])
        pbank = [4 + 2 * i, 5 + 2 * i]
        for kc in range(16):
            bk = pbank[kc // 8]
            pv = c.ps[bk][:].bitcast(BF16)
            o = (kc % 8) * 128
            S.op("pe", lambda e, pv=pv, o=o, kc=kc, i=i: e.transpose(out=pv[:, o:o + 128], in_=xs[i][:, kc * 128:(kc + 1) * 128], identity=c.ident[:]),
                 reads=[xsb[i], c.cb], writes=[c.psb[bk]], inc=(kc % 8 == 7))
        for hh in range(2):
            bk = pbank[hh]
            pv = c.ps[bk][:].bitcast(BF16)[:, 0:1024].rearrange("p (k t) -> p k t", t=128)
            g = c.ppt[:, goff + 8 * hh: goff + 8 * hh + 8].unsqueeze(2).to_broadcast([128, 8, 128])
            S.op("dve", lambda e, pv=pv, g=g, hh=hh, tt=tt: e.tensor_tensor(out=c.XT[:, 8 * hh:8 * hh + 8, tt * 128:(tt + 1) * 128], in0=pv, in1=g, op=ALU.mult),
                 reads=[c.psb[bk], c.cb], writes=[c.xtb[tt]])


def phase_gemm(c, L):
    S = c.S
    A = c.ARENA
    A = A + 29 * 1024
    ns = 72 if L == 0 else 48
    ns = min(ns, NS_MAX)
    ntail = 8 if L == 0 else 16
    NW = 3
    wt = [c.sbt([128, 16, 128], BF16, A + i * 4096) for i in range(NW)]
    wtb = [Buf() for _ in range(NW)]
    stg = [c.sbt([128, S_LEN], BF16, A + NW * 4096 + i * 4096) for i in range(2)]
    stgb = [Buf() for _ in range(2)]
    wtl = c.sbt([128, 16, ntail], BF16, A + NW * 4096 + 8192)
    wtlb = Buf()
    stl = c.sbt([ntail, S_LEN], F32, A + NW * 4096 + 8192 + 1024)
    stlb = Buf()
    allx = list(c.xtb)
    dstb = Buf()
    for s in range(min(NW, ns)):
        S.dma("pool", wt[s % NW][:], c.win[L][s], writes=[wtb[s % NW]])
    S.dma("pool", wtl[:], c.wtail[L], writes=[wtlb])
    for s in range(ns):
        wi = s % NW
        for half in range(2):
            banks = [2 * ((2 * s + half) % 2), 2 * ((2 * s + half) % 2) + 1]
            for nb in range(2):
                bk = banks[nb]
                t0 = half * 1024 + nb * 512
                for kc in range(16):
                    S.op("pe", lambda e, bk=bk, wi=wi, kc=kc, t0=t0: e.matmul(c.ps[bk][:, :], lhsT=wt[wi][:, kc, :], rhs=c.XT[:, kc, t0:t0 + 512], start=(kc == 0), stop=(kc == 15)),
                         reads=[wtb[wi]] + allx[t0 // 128: t0 // 128 + 4], writes=[c.psb[bk]], inc=(kc == 15))
                eng = "act" if nb == 0 else "dve"
                if eng == "act":
                    S.op("act", lambda e, bk=bk, t0=t0, s=s: e.activation(out=stg[s % 2][:, t0:t0 + 512], in_=c.ps[bk][:, :], func=AF.Copy), reads=[c.psb[bk]], writes=[stgb[s % 2]])
                else:
                    S.op("dve", lambda e, bk=bk, t0=t0, s=s: e.tensor_copy(out=stg[s % 2][:, t0:t0 + 512], in_=c.ps[bk][:, :]), reads=[c.psb[bk]], writes=[stgb[s % 2]])
        S.dma("sp", c.projT[L][s], stg[s % 2][:], reads=[stgb[s % 2]], writes=[dstb])
        if s + NW < ns:
            S.dma("pool", wt[wi][:], c.win[L][s + NW], writes=[wtb[wi]])
    for nb in range(4):
        bk = nb % 2
        t0 = nb * 512
        for kc in range(16):
            S.op("pe", lambda e, bk=bk, kc=kc, t0=t0: e.matmul(c.ps[bk][0:ntail, :], lhsT=wtl[:, kc, :], rhs=c.XT[:, kc, t0:t0 + 512], start=(kc == 0), stop=(kc == 15)),
                 reads=[wtlb] + allx[t0 // 128: t0 // 128 + 4], writes=[c.psb[bk]], inc=(kc == 15))
        S.op("act", lambda e, bk=bk, t0=t0: e.activation(out=stl[:, t0:t0 + 512], in_=c.ps[bk][0:ntail, :], func=AF.Copy), reads=[c.psb[bk]], writes=[stlb])
    S.dma("sp", c.ptail[L], stl[:], reads=[stlb], writes=[dstb])


def phase_outp(c, L, src):
    S = c.S
    A = c.ARENA
    wo = [c.sbt([128, 16, 512], BF16, A + i * 16384) for i in range(2)]
    wob = [Buf() for _ in range(2)]
    xr = [c.sbt([128, 512], F32, A + 32768 + i * 2048) for i in range(4)]
    xrb = [Buf() for _ in range(4)]
    ally = list(c.ytb)
    if not hasattr(c, "outb"):
        c.outb = [[Buf() for _ in range(4)] for _ in range(NT)]
    S.dma("pool", wo[0][:], c.wout[L][0], writes=[wob[0]])
    it = 0
    for cbk in range(4):
        wi = cbk % 2
        if cbk + 1 < 4:
            S.dma("pool", wo[(cbk + 1) % 2][:], c.wout[L][cbk + 1], writes=[wob[(cbk + 1) % 2]])
        for tt in range(NT):
            xi = it % 4
            bk = it % 4
            it += 1
            rd = [c.outb[tt][cbk]] if src is c.out else []
            S.dma("act", xr[xi][:], src[tt * 128:(tt + 1) * 128, cbk * 512:(cbk + 1) * 512], reads=rd, writes=[xrb[xi]])
            for kc in range(16):
                S.op("pe", lambda e, bk=bk, wi=wi, kc=kc, tt=tt: e.matmul(c.ps[bk][:, :], lhsT=c.YT[:, kc, tt * 128:(tt + 1) * 128], rhs=wo[wi][:, kc, :], start=(kc == 0), stop=(kc == 15)),
                     reads=[wob[wi]] + ally, writes=[c.psb[bk]], inc=(kc == 15))
            S.op("dve", lambda e, bk=bk, xi=xi: e.tensor_tensor(out=xr[xi][:], in0=c.ps[bk][:, :], in1=xr[xi][:], op=ALU.add), reads=[c.psb[bk], xrb[xi]], writes=[xrb[xi]])
            S.dma("sp", c.out[tt * 128:(tt + 1) * 128, cbk * 512:(cbk + 1) * 512], xr[xi][:], reads=[xrb[xi]], writes=[c.outb[tt][cbk]])


def rep_rows(c, dst, dstb, rows, rowsb, h, nrows, bank0=4, p0=0):
    S = c.S
    for nb in range(4):
        bk = bank0 + (nb % 2)
        S.op("pe", lambda e, bk=bk, nb=nb: e.matmul(c.ps[bk][:, :], lhsT=c.sel[p0:p0 + nrows, h, :], rhs=rows[p0:p0 + nrows, nb * 512:(nb + 1) * 512], start=True, stop=True),
             reads=[rowsb, c.cb], writes=[c.psb[bk]])
        S.op("act", lambda e, bk=bk, nb=nb: e.activation(out=dst[:, nb * 512:(nb + 1) * 512], in_=c.ps[bk][:, :], func=AF.Copy), reads=[c.psb[bk]], writes=[dstb])


def cols_from_rows(c, dst, dstb, rows, rowsb, nrows, bank=6):
    S = c.S
    for j in range(NT):
        S.op("pe", lambda e, j=j: e.transpose(out=c.ps[bank][:, j * nrows:(j + 1) * nrows], in_=rows[0:nrows, j * 128:(j + 1) * 128], identity=c.identf[0:nrows, 0:nrows]),
             reads=[rowsb, c.cb], writes=[c.psb[bank]], inc=(j == NT - 1))
    S.op("dve", lambda e: e.tensor_copy(out=dst[:].rearrange("p j r -> p (j r)"), in_=c.ps[bank][:, 0:NT * nrows]), reads=[c.psb[bank]], writes=[dstb])


def sumsq_rstd(c, dst, dstb, srcT, srcb, sq, sqb, scale, bias_ap, banks=(4, 5), c0=0, c1=S_LEN):
    S = c.S
    S.op("pool", lambda e: e.tensor_tensor(out=sq[:, c0:c1], in0=srcT[:, c0:c1], in1=srcT[:, c0:c1], op=ALU.mult), reads=[srcb], writes=[sqb])
    for nb in range(c0 // 512, c1 // 512):
        bk = banks[nb % 2]
        S.op("pe", lambda e, bk=bk, nb=nb: e.matmul(c.ps[bk][:, :], lhsT=c.ones[:], rhs=sq[:, nb * 512:(nb + 1) * 512], start=True, stop=True), reads=[sqb, c.cb], writes=[c.psb[bk]])
        S.op("act", lambda e, bk=bk, nb=nb: e.activation(out=dst[:, nb * 512:(nb + 1) * 512], in_=c.ps[bk][:, :], func=AF.Ln, scale=scale, bias=bias_ap), reads=[c.psb[bk], c.cb], writes=[dstb])
    S.op("act", lambda e: e.activation(out=dst[:, c0:c1], in_=dst[:, c0:c1], func=AF.Exp, scale=-0.5), reads=[dstb], writes=[dstb])


def to_tokmajor(c, dst, dstb, srcT, srcb, bank=6, ncols=128):
    S = c.S
    for g in range(2):
        bk = bank + g
        pv = c.ps[bk][:].bitcast(BF16)
        for jj in range(8):
            j = g * 8 + jj
            S.op("pe", lambda e, pv=pv, jj=jj, j=j: e.transpose(out=pv[:, jj * 128:jj * 128 + ncols], in_=srcT[:, j * 128:(j + 1) * 128], identity=c.ident[:]),
                 reads=[srcb, c.cb], writes=[c.psb[bk]], inc=(jj == 7))
        S.op("act", lambda e, pv=pv, g=g: e.activation(out=dst[:, 8 * g:8 * g + 8, 0:ncols], in_=pv[:, 0:1024].rearrange("p (j d) -> p j d", d=128)[:, :, 0:ncols], func=AF.Copy),
             reads=[c.psb[bk]], writes=[dstb])


def phase_mix_a(c):
    S = c.S
    al = Arena(c)
    qT = [al([128, S_LEN], BF16, 4096) for _ in range(2)]
    kT = [al([128, S_LEN], BF16, 4096) for _ in range(2)]
    vT = [al([128, S_LEN], BF16, 4096) for _ in range(2)]
    zT = [al([128, S_LEN], BF16, 4096) for _ in range(2)]
    lb = [[Buf() for _ in range(4)] for _ in range(2)]
    bt = [al([128, 640], BF16, 1280) for _ in range(2)]
    btb = [Buf() for _ in range(2)]
    sq = al([128, S_LEN], BF16, 4096); sqb = Buf()
    rq = al([128, S_LEN], F32, 8192); rqb = Buf()
    qh = [al([128, S_LEN], BF16, 4096) for _ in range(2)]; qhb = [Buf() for _ in range(2)]
    kh = [al([128, S_LEN], BF16, 4096) for _ in range(2)]; khb = [Buf() for _ in range(2)]
    Vt = [al([128, 16, 128], BF16, 4096) for _ in range(2)]; Vtb = [Buf() for _ in range(2)]
    sz = [al([128, S_LEN], BF16, 4096) for _ in range(2)]; szb = [Buf() for _ in range(2)]
    pT = [al([128, 640], BF16, 1280) for _ in range(2)]; pTb = [Buf() for _ in range(2)]
    rd = [al([128, 128], F32, 512) for _ in range(2)]; rdb = [Buf() for _ in range(2)]
    eps128 = c.epst[:, 1:2]
    epsb = c.epst[:, 0:1]

    def load(h):
        i = h % 2
        for j, (t, base) in enumerate(((qT, 0), (kT, 8), (vT, 16), (zT, 24))):
            S.dma("sp", t[i][:], c.projT[0][base + h], writes=[lb[i][j]])
        S.dma("pool", bt[i][:], c.bt[h], writes=[btb[i]])

    def prologue(h):
        i = h % 2
        th = []

        def norm(srcT, srcb, dst, dstb, scale, bias_ap, gcol):
            th.append(lambda: S.op("pool", lambda e: e.tensor_tensor(out=sq[:], in0=srcT[:], in1=srcT[:], op=ALU.mult), reads=[srcb], writes=[sqb]))
            for nb in range(4):
                bk = 6 + (nb % 2)
                def f(nb=nb, bk=bk):
                    S.op("pe", lambda e: e.matmul(c.ps[bk][:, :], lhsT=c.ones[:], rhs=sq[:, nb * 512:(nb + 1) * 512], start=True, stop=True), reads=[sqb, c.cb], writes=[c.psb[bk]])
                    S.op("act", lambda e: e.activation(out=rq[:, nb * 512:(nb + 1) * 512], in_=c.ps[bk][:, :], func=AF.Ln, scale=scale, bias=bias_ap), reads=[c.psb[bk], c.cb], writes=[rqb])
                th.append(f)
            th.append(lambda: S.op("act", lambda e: e.activation(out=rq[:], in_=rq[:], func=AF.Exp, scale=-0.5), reads=[rqb], writes=[rqb]))
            th.append(lambda: S.op("dve", lambda e: e.scalar_tensor_tensor(out=dst[:], in0=srcT[:], scalar=c.ppt[:, gcol:gcol + 1], in1=rq[:], op0=ALU.mult, op1=ALU.mult), reads=[srcb, rqb, c.cb], writes=[dstb]))
        norm(qT[i], lb[i][0], qh[i], qhb[i], 1.0, eps128, PP_QN)
        norm(kT[i], lb[i][1], kh[i], khb[i], 1.0 / 128, epsb, PP_KN)
        for g in range(2):
            def f(g=g):
                bk = 6 + g
                pv = c.ps[bk][:].bitcast(BF16)
                for jj in range(8):
                    j = g * 8 + jj
                    S.op("pe", lambda e, jj=jj, j=j: e.transpose(out=pv[:, jj * 128:(jj + 1) * 128], in_=vT[i][:, j * 128:(j + 1) * 128], identity=c.ident[:]),
                         reads=[lb[i][2], c.cb], writes=[c.psb[bk]], inc=(jj == 7))
                S.op("act", lambda e: e.activation(out=Vt[i][:, 8 * g:8 * g + 8, :], in_=pv[:, 0:1024].rearrange("p (j d) -> p j d", d=128), func=AF.Copy),
                     reads=[c.psb[bk]], writes=[Vtb[i]])
            th.append(f)
        th.append(lambda: S.op("act", lambda e: e.activation(out=sz[i][:], in_=zT[i][:], func=AF.Silu), reads=[lb[i][3]], writes=[szb[i]]))
        return th

    def stage1(h, n, k):
        i = h % 2
        jb0 = max(0, 4 - n)
        w0 = jb0 * 128
        bks = (0, 1) if k == 0 else (2, 3)
        for jb in range(jb0, 5):
            kb = n - 4 + jb
            bk = bks[0] if jb < 4 else bks[1]
            oo = (jb % 4) * 128
            S.op("pe", lambda e, bk=bk, oo=oo, kb=kb: e.matmul(c.ps[bk][:, oo:oo + 128], lhsT=kh[i][:, kb * 128:(kb + 1) * 128], rhs=qh[i][:, n * 128:(n + 1) * 128], start=True, stop=False),
                 reads=[khb[i], qhb[i]], writes=[c.psb[bk]], inc=False)
            S.op("pe", lambda e, bk=bk, oo=oo, jb=jb: e.matmul(c.ps[bk][:, oo:oo + 128], lhsT=c.ident[:], rhs=bt[i][:, jb * 128:(jb + 1) * 128], start=False, stop=True),
                 reads=[btb[i], c.cb], writes=[c.psb[bk]], inc=(jb == 3 or jb == 4))
        if jb0 < 4:
            S.op("act", lambda e: e.activation(out=pT[k][:, w0:512], in_=c.ps[bks[0]][:, w0:512], func=AF.Exp), reads=[c.psb[bks[0]]], writes=[pTb[k]])
        S.op("act", lambda e: e.activation(out=pT[k][:, 512:640], in_=c.ps[bks[1]][:, 0:128], func=AF.Exp), reads=[c.psb[bks[1]]], writes=[pTb[k]])

    def stage2(h, n, k):
        i = h % 2
        jb0 = max(0, 4 - n)
        bkC = 4 + k
        nj = 5 - jb0
        for idx, jb in enumerate(range(jb0, 5)):
            kb = n - 4 + jb
            S.op("pe", lambda e, kb=kb, jb=jb, idx=idx: e.matmul(c.ps[bkC][:, 0:128], lhsT=Vt[i][:, kb, :], rhs=pT[k][:, jb * 128:(jb + 1) * 128], start=(idx == 0), stop=(idx == nj - 1)),
                 reads=[Vtb[i], pTb[k]], writes=[c.psb[bkC]], inc=False)
        for idx, jb in enumerate(range(jb0, 5)):
            S.op("pe", lambda e, jb=jb, idx=idx: e.matmul(c.ps[bkC][:, 128:256], lhsT=c.ones[:], rhs=pT[k][:, jb * 128:(jb + 1) * 128], start=(idx == 0), stop=(idx == nj - 1)),
                 reads=[c.cb, pTb[k]], writes=[c.psb[bkC]], inc=(idx == nj - 1))
        S.op("dve", lambda e: e.reciprocal(out=rd[k][:], in_=c.ps[bkC][:, 128:256]), reads=[c.psb[bkC]], writes=[rdb[k]])
        S.op("pool", lambda e: e.tensor_tensor(out=rd[k][:], in0=rd[k][:], in1=sz[i][:, n * 128:(n + 1) * 128], op=ALU.mult), reads=[rdb[k], szb[i]], writes=[rdb[k]])
        S.op("dve", lambda e: e.tensor_tensor(out=c.YT[:, h, n * 128:(n + 1) * 128], in0=c.ps[bkC][:, 0:128], in1=rd[k][:], op=ALU.mult), reads=[c.psb[bkC], rdb[k]], writes=[c.ytb[h]])

    load(0)
    for t in prologue(0):
        t()
    its = [(h, n) for h in range(8) for n in range(NT)]
    pend = []
    for idx in range(len(its) + 1):
        if idx < len(its):
            h, n = its[idx]
            if n == 0 and h + 1 < 8:
                load(h + 1)
                pend = prologue(h + 1)
            stage1(h, n, idx % 2)
        if idx >= 1:
            h2, n2 = its[idx - 1]
            stage2(h2, n2, (idx - 1) % 2)
            if n2 >= 2:
                for _ in range(2):
                    if pend:
                        pend.pop(0)()
            if n2 == NT - 1:
                while pend:
                    pend.pop(0)()


def phase_mix_b(c):
    S = c.S
    al = Arena(c)
    gi = al([4, S_LEN], F32, 8192); gib = Buf()
    gf = al([4, S_LEN], F32, 8192); gfb = Buf()
    bb = al([4, S_LEN], F32, 8192); bbb = Buf()
    eb = al([4, S_LEN], F32, 8192); ebb = Buf()
    ek = gi; ekb = gib
    nbf = al([4, 1], F32, 16); nbfb = Buf()
    ekc = al([128, 16, 4], F32, 256); ekcb = Buf()
    S.dma("sp", gi[:], c.ptail[0][0:4, :], writes=[gib])
    S.dma("sp", gf[:], c.ptail[0][4:8, :], writes=[gfb])
    S.op("dve", lambda e: e.tensor_scalar(out=nbf[:], in0=c.hpt[0:4, 1:2], scalar1=-1.0, scalar2=None, op0=ALU.mult), reads=[c.cb], writes=[nbfb])
    S.op("act", lambda e: e.activation(out=gi[:], in_=gi[:], func=AF.Identity, bias=c.hpt[0:4, 0:1]), reads=[gib, c.cb], writes=[gib])
    S.op("act", lambda e: e.activation(out=gf[:], in_=gf[:], func=AF.Exp, scale=-1.0, bias=nbf[:]), reads=[gfb, nbfb], writes=[gfb])
    S.op("act", lambda e: e.activation(out=gf[:], in_=gf[:], func=AF.Ln, bias=c.epst[0:4, 2:3]), reads=[gfb, c.cb], writes=[gfb])
    S.op("dve", lambda e: e.tensor_scalar(out=gf[:], in0=gf[:], scalar1=-1.0, scalar2=None, op0=ALU.mult), reads=[gfb], writes=[gfb])
    S.op("dve", lambda e: e.tensor_tensor_scan(out=bb[:], data0=c.cmask[0:4, :], data1=gf[:], initial=0.0, op0=ALU.mult, op1=ALU.add), reads=[gfb, c.cb], writes=[bbb])
    S.op("act", lambda e: e.activation(out=eb[:], in_=bb[:], func=AF.Exp), reads=[bbb], writes=[ebb])
    S.op("dve", lambda e: e.tensor_tensor(out=ek[:], in0=gi[:], in1=bb[:], op=ALU.subtract), reads=[gib, bbb], writes=[ekb])
    S.op("act", lambda e: e.activation(out=ek[:], in_=ek[:], func=AF.Exp), reads=[ekb], writes=[ekb])
    cols_from_rows(c, ekc, ekcb, ek, ekb, 4)
    EB = al([128, S_LEN], F32, 8192); EBb = Buf()
    EK = al([128, S_LEN], F32, 8192); EKb = Buf()
    ld = [[al([128, S_LEN], BF16, 4096) for _ in range(2)] for _ in range(5)]
    ldb = [[Buf() for _ in range(2)] for _ in range(5)]
    G = [al([128, S_LEN], BF16, 4096) for _ in range(2)]; Gb = [Buf() for _ in range(2)]
    Vx = al([128, 16, 258], BF16, 16 * 258 * 2); Vxb = Buf()
    qs = [al([128, 2, 128], BF16, 512) for _ in range(2)]; qsb = [Buf() for _ in range(2)]
    ksT = [al([128, 2, 128], BF16, 512) for _ in range(2)]; ksTb = [Buf() for _ in range(2)]
    kst = [al([128, 256], BF16, 512) for _ in range(2)]; kstb = [Buf() for _ in range(2)]
    ST = [al([128, 128], BF16, 256) for _ in range(2)]; STb = [Buf() for _ in range(2)]
    Cf = al([128, 2, 258], F32, 2 * 258 * 4); Cfb = Buf()
    Cb = al([128, 2, 256], BF16, 1024); Cbb = Buf()
    Nr = al([128, 2, 128], BF16, 512); Nrb = Buf()
    dd = [al([128, 128], F32, 512) for _ in range(2)]; ddb = [Buf() for _ in range(2)]
    hh_ = [al([128, 2, 128], F32, 1024) for _ in range(2)]; hhb = [Buf() for _ in range(2)]
    base = (32, 40, 48, 56, 64)
    for h in range(4):
        for j in range(5):
            for cc in range(2):
                S.dma("sp", ld[j][cc][:], c.projT[0][base[j] + 2 * h + cc], writes=[ldb[j][cc]])
        rep_rows(c, EB, EBb, eb, ebb, h, 4)
        rep_rows(c, EK, EKb, ek, ekb, h, 4)
        for cc in range(2):
            S.op("act", lambda e, cc=cc: e.activation(out=ld[3][cc][:], in_=ld[3][cc][:], func=AF.Sigmoid), reads=[ldb[3][cc]], writes=[ldb[3][cc]])
            S.op("act", lambda e, cc=cc: e.activation(out=ld[4][cc][:], in_=ld[4][cc][:], func=AF.Silu), reads=[ldb[4][cc]], writes=[ldb[4][cc]])
            S.op("pool", lambda e, cc=cc: e.tensor_tensor(out=G[cc][:], in0=ld[3][cc][:], in1=ld[4][cc][:], op=ALU.mult), reads=[ldb[3][cc], ldb[4][cc]], writes=[Gb[cc]])
        S.op("pool", lambda e: e.memset(Vx[:, :, 256:258], 1.0), writes=[Vxb])
        for cc in range(2):
            for g in range(2):
                bk = 6 + g
                pv = c.ps[bk][:].bitcast(BF16)
                for jj in range(8):
                    j = g * 8 + jj
                    S.op("pe", lambda e, pv=pv, jj=jj, j=j, cc=cc: e.transpose(out=pv[:, jj * 128:(jj + 1) * 128], in_=ld[2][cc][:, j * 128:(j + 1) * 128], identity=c.ident[:]),
                         reads=[ldb[2][cc], c.cb], writes=[c.psb[bk]], inc=(jj == 7))
                S.op("act", lambda e, pv=pv, g=g, cc=cc: e.activation(out=Vx[:, 8 * g:8 * g + 8, cc * 128:(cc + 1) * 128], in_=pv[:, 0:1024].rearrange("p (j d) -> p j d", d=128), func=AF.Copy),
                     reads=[c.psb[bk]], writes=[Vxb])
        S.op("pool", lambda e: e.memset(Cf[:], 0.0), writes=[Cfb])
        S.op("pool", lambda e: e.memset(Cb[:], 0.0), writes=[Cbb])
        S.op("pool", lambda e: e.memset(Nr[:], 0.0), writes=[Nrb])
        for n in range(NT):
            k = n % 2
            tsl = slice(n * 128, (n + 1) * 128)
            for cc in range(2):
                S.op("dve", lambda e, k=k, cc=cc, tsl=tsl: e.scalar_tensor_tensor(out=qs[k][:, cc, :], in0=ld[0][cc][:, tsl], scalar=1.0 / 16.0, in1=EB[:, tsl], op0=ALU.mult, op1=ALU.mult), reads=[ldb[0][cc], EBb], writes=[qsb[k]])
                S.op("pool", lambda e, k=k, cc=cc, tsl=tsl: e.tensor_tensor(out=ksT[k][:, cc, :], in0=ld[1][cc][:, tsl], in1=EK[:, tsl], op=ALU.mult), reads=[ldb[1][cc], EKb], writes=[ksTb[k]])
            bkA = 0 + k
            pv = c.ps[bkA][:].bitcast(BF16)
            for cc in range(2):
                S.op("pe", lambda e, pv=pv, cc=cc, k=k: e.transpose(out=pv[:, cc * 128:(cc + 1) * 128], in_=ksT[k][:, cc, :], identity=c.ident[:]), reads=[ksTb[k], c.cb], writes=[c.psb[bkA]], inc=(cc == 1))
            S.op("act", lambda e, pv=pv, k=k: e.activation(out=kst[k][:], in_=pv[:, 0:256], func=AF.Copy), reads=[c.psb[bkA]], writes=[kstb[k]])
            bkS = 2 + k
            for cc in range(2):
                S.op("pe", lambda e, bkS=bkS, cc=cc, k=k: e.matmul(c.ps[bkS][:, 0:128], lhsT=ksT[k][:, cc, :], rhs=qs[k][:, cc, :], start=(cc == 0), stop=(cc == 1)), reads=[ksTb[k], qsb[k]], writes=[c.psb[bkS]], inc=(cc == 1))
            S.op("dve", lambda e, bkS=bkS, k=k: e.tensor_tensor(out=ST[k][:], in0=c.ps[bkS][:, 0:128], in1=c.m_iu[:], op=ALU.mult), reads=[c.psb[bkS], c.cb], writes=[STb[k]])
            bkN = 4 + k
            for cv in range(2):
                for cc in range(2):
                    S.op("pe", lambda e, bkN=bkN, cv=cv, cc=cc, k=k: e.matmul(c.ps[bkN][:, cv * 128:(cv + 1) * 128], lhsT=Cb[:, cc, cv * 128:(cv + 1) * 128], rhs=qs[k][:, cc, :], start=(cc == 0), stop=False), reads=[Cbb, qsb[k]], writes=[c.psb[bkN]], inc=False)
                S.op("pe", lambda e, bkN=bkN, cv=cv, k=k, n=n: e.matmul(c.ps[bkN][:, cv * 128:(cv + 1) * 128], lhsT=Vx[:, n, cv * 128:(cv + 1) * 128], rhs=ST[k][:], start=False, stop=True), reads=[Vxb, STb[k]], writes=[c.psb[bkN]], inc=False)
            for cc in range(2):
                S.op("pe", lambda e, bkN=bkN, cc=cc, k=k: e.matmul(c.ps[bkN][:, 256:384], lhsT=Nr[:, cc, :], rhs=qs[k][:, cc, :], start=(cc == 0), stop=False), reads=[Nrb, qsb[k]], writes=[c.psb[bkN]], inc=False)
            S.op("pe", lambda e, bkN=bkN, k=k: e.matmul(c.ps[bkN][:, 256:384], lhsT=c.ones[:], rhs=ST[k][:], start=False, stop=True), reads=[c.cb, STb[k]], writes=[c.psb[bkN]])
            S.op("dve", lambda e, bkN=bkN, k=k: e.tensor_scalar(out=dd[k][:], in0=c.ps[bkN][:, 256:384], scalar1=-1.0, scalar2=1.0, op0=ALU.mult, op1=ALU.max), reads=[c.psb[bkN]], writes=[ddb[k]])
            S.op("dve", lambda e, bkN=bkN, k=k: e.tensor_tensor(out=dd[k][:], in0=dd[k][:], in1=c.ps[bkN][:, 256:384], op=ALU.max), reads=[c.psb[bkN], ddb[k]], writes=[ddb[k]])
            S.op("dve", lambda e, k=k: e.reciprocal(out=dd[k][:], in_=dd[k][:]), reads=[ddb[k]], writes=[ddb[k]])
            S.op("dve", lambda e, bkN=bkN, k=k: e.tensor_tensor(out=hh_[k][:], in0=c.ps[bkN][:, 0:256].rearrange("p (c t) -> p c t", t=128), in1=dd[k][:].unsqueeze(1).to_broadcast([128, 2, 128]), op=ALU.mult), reads=[c.psb[bkN], ddb[k]], writes=[hhb[k]])
            for cv in range(2):
                S.op("pool", lambda e, k=k, cv=cv, h=h, tsl=tsl: e.tensor_tensor(out=c.YT[:, 8 + 2 * h + cv, tsl], in0=hh_[k][:, cv, :], in1=G[cv][:, tsl], op=ALU.mult), reads=[hhb[k], Gb[cv]], writes=[c.ytb[8 + 2 * h + cv]])
            if n + 1 < NT:
                ebl = EB[:, n * 128 + 127:n * 128 + 128]
                eblp = EB[:, max(n - 1, 0) * 128 + 127:max(n - 1, 0) * 128 + 128]
                for cc in range(2):
                    bkU = 6 + cc
                    S.op("pe", lambda e, bkU=bkU, cc=cc, k=k, n=n: e.matmul(c.ps[bkU][:, 0:258], lhsT=kst[k][:, cc * 128:(cc + 1) * 128], rhs=Vx[:, n, :], start=True, stop=True), reads=[kstb[k], Vxb], writes=[c.psb[bkU]])
                    S.op("dve", lambda e, cc=cc, bkU=bkU, eblp=eblp: e.scalar_tensor_tensor(out=Cf[:, cc, :], in0=Cf[:, cc, :], scalar=eblp, in1=c.ps[bkU][:, 0:258], op0=ALU.mult, op1=ALU.add), reads=[c.psb[bkU], Cfb, EBb], writes=[Cfb])
                S.op("act", lambda e, ebl=ebl: e.activation(out=Cb[:], in_=Cf[:, :, 0:256], func=AF.Copy, scale=ebl), reads=[Cfb, EBb], writes=[Cbb])
                S.op("act", lambda e, ebl=ebl: e.activation(out=Nr[:], in_=Cf[:, :, 256:257].to_broadcast([128, 2, 128]), func=AF.Copy, scale=ebl), reads=[Cfb, EBb], writes=[Nrb])


def conv4(c, acc, accb, xpad, xpadb, wcol0, bias_ap=None, pad=3, c0=0, c1=S_LEN):
    S = c.S
    w = lambda j: c.ppt[:, wcol0 + j:wcol0 + j + 1]
    p3 = pad - 3
    if bias_ap is not None:
        S.op("act", lambda e: e.activation(out=acc[:, c0:c1], in_=xpad[:, pad + c0:pad + c1], func=AF.Identity, scale=w(3), bias=bias_ap), reads=[xpadb, c.cb], writes=[accb])
    else:
        S.op("act", lambda e: e.activation(out=acc[:, c0:c1], in_=xpad[:, pad + c0:pad + c1], func=AF.Copy, scale=w(3)), reads=[xpadb, c.cb], writes=[accb])
    for j in range(3):
        S.op("dve", lambda e, j=j: e.scalar_tensor_tensor(out=acc[:, c0:c1], in0=xpad[:, p3 + j + c0:p3 + j + c1], scalar=w(j), in1=acc[:, c0:c1], op0=ALU.mult, op1=ALU.add), reads=[xpadb, accb, c.cb], writes=[accb])


def phase_mix_c(c):
    S = c.S
    al = Arena(c)
    xp = [al([128, 3 + S_LEN + 1], BF16, 4104) for _ in range(2)]; xpb = [Buf() for _ in range(2)]
    zt = [al([128, S_LEN], BF16, 4096) for _ in range(2)]; ztb = [Buf() for _ in range(2)]
    gw = [al([128, 256], BF16, 512) for _ in range(2)]; gwb = [Buf() for _ in range(2)]
    acc2 = [al([128, S_LEN], F32, 8192) for _ in range(2)]; accb2 = [Buf() for _ in range(2)]
    xcb2 = [al([128, S_LEN], BF16, 4096) for _ in range(2)]; xcbb2 = [Buf() for _ in range(2)]
    r2 = [al([128, S_LEN], F32, 8192) for _ in range(2)]; rb2 = [Buf() for _ in range(2)]
    i2 = [al([128, S_LEN], F32, 8192) for _ in range(2)]; ib2 = [Buf() for _ in range(2)]
    a2 = [al([128, S_LEN], F32, 8192) for _ in range(2)]; ab2 = [Buf() for _ in range(2)]
    hs2 = [al([128, S_LEN], F32, 8192) for _ in range(2)]; hsb2 = [Buf() for _ in range(2)]
    cc_ = al([128, 8], F32, 32); ccb = Buf()
    S.op("act", lambda e: e.activation(out=cc_[:], in_=c.ppt[:, PP_LAM:PP_LAM + 8], func=AF.Exp, scale=-1.0), reads=[c.cb], writes=[ccb])
    S.op("act", lambda e: e.activation(out=cc_[:], in_=cc_[:], func=AF.Ln, bias=c.epst[:, 2:3]), reads=[ccb, c.cb], writes=[ccb])
    S.op("dve", lambda e: e.tensor_scalar(out=cc_[:], in0=cc_[:], scalar1=-8.0, scalar2=None, op0=ALU.mult), reads=[ccb], writes=[ccb])
    for i in range(2):
        S.op("pool", lambda e, i=i: e.memset(xp[i][:, 0:4], 0.0), writes=[xpb[i]])

    def load(n):
        i = n % 2
        S.dma("sp", xp[i][:, 3:3 + S_LEN], c.projT[1][n], writes=[xpb[i]])
        S.dma("sp", zt[i][:], c.projT[1][8 + n], writes=[ztb[i]])
        S.dma("pool", gw[i][:], c.gatew[n], writes=[gwb[i]])
    load(0)
    for n in range(8):
        i = n % 2
        acc, accb, xcb, xcbb = acc2[i], accb2[i], xcb2[i], xcbb2[i]
        r_, rb, i_, ib, a_, ab, hs, hsb = r2[i], rb2[i], i2[i], ib2[i], a2[i], ab2[i], hs2[i], hsb2[i]
        if n + 1 < 8:
            load(n + 1)
        conv4(c, acc, accb, xp[i], xpb[i], PP_CCW + 4 * n, bias_ap=c.ppt[:, PP_CCB + n:PP_CCB + n + 1])
        S.op("pool", lambda e, xcb=xcb, acc=acc: e.tensor_copy(out=xcb[:], in_=acc[:]), reads=[accb], writes=[xcbb])
        for part, dst, dstb, bcol in ((0, r_, rb, PP_GBR), (1, i_, ib, PP_GBI)):
            for nb in range(4):
                bk = (part * 4 + nb) % 8
                S.op("pe", lambda e, bk=bk, part=part, nb=nb, i=i, xcb=xcb: e.matmul(c.ps[bk][:, :], lhsT=gw[i][:, part * 128:(part + 1) * 128], rhs=xcb[:, nb * 512:(nb + 1) * 512], start=True, stop=True), reads=[gwb[i], xcbb], writes=[c.psb[bk]])
                S.op("act", lambda e, bk=bk, nb=nb, dst=dst, bcol=bcol, n=n: e.activation(out=dst[:, nb * 512:(nb + 1) * 512], in_=c.ps[bk][:, :], func=AF.Sigmoid, bias=c.ppt[:, bcol + n:bcol + n + 1]), reads=[c.psb[bk], c.cb], writes=[dstb])
        S.op("act", lambda e, n=n, a_=a_, r_=r_: e.activation(out=a_[:], in_=r_[:], func=AF.Exp, scale=cc_[:, n:n + 1]), reads=[rb, ccb], writes=[ab])
        S.op("pool", lambda e, a_=a_, r_=r_: e.tensor_tensor(out=r_[:], in0=a_[:], in1=a_[:], op=ALU.mult), reads=[ab, rb], writes=[rb])
        S.op("act", lambda e, r_=r_: e.activation(out=r_[:], in_=r_[:], func=AF.Sqrt, scale=-1.0, bias=c.epst[:, 2:3]), reads=[rb, c.cb], writes=[rb])
        S.op("dve", lambda e, i_=i_, acc=acc: e.tensor_tensor(out=i_[:], in0=i_[:], in1=acc[:], op=ALU.mult), reads=[ib, accb], writes=[ib])
        S.op("dve", lambda e, i_=i_, r_=r_: e.tensor_tensor(out=i_[:], in0=i_[:], in1=r_[:], op=ALU.mult), reads=[ib, rb], writes=[ib])
        S.op("dve", lambda e, hs=hs, a_=a_, i_=i_: e.tensor_tensor_scan(out=hs[:], data0=a_[:], data1=i_[:], initial=0.0, op0=ALU.mult, op1=ALU.add), reads=[ab, ib], writes=[hsb])
        S.op("act", lambda e, i=i: e.activation(out=zt[i][:], in_=zt[i][:], func=AF.Silu), reads=[ztb[i]], writes=[ztb[i]])
        S.op("pool", lambda e, i=i, n=n, hs=hs: e.tensor_tensor(out=c.YT[:, n, :], in0=hs[:], in1=zt[i][:], op=ALU.mult), reads=[hsb, ztb[i]], writes=[c.ytb[n]])


def phase_mix_d(c):
    S = c.S
    al = Arena(c, segs=[(0, 65536), (65536 + 32768, 131072), (c.ARENA, SBUF_BYTES)])
    G = 4
    NGR = NT // G
    odT, odT_off = al.at([128, S_LEN], F32, 8192); odTb = Buf()
    RD = al([40, S_LEN], F32, 8192); RDb = Buf(); RBb = Buf()
    nea = al([8, 1], F32, 32); neab = Buf()
    cols = al([128, 16, 16], F32, 1024); colsb = Buf()
    c3 = al([128, 16, 8, 2], F32, 1024); c3b = Buf()
    dlr2 = [al([128, 16], F32, 64) for _ in range(2)]; dlrb2 = [Buf(), Buf()]
    tmpc = al([128, 16, 8], F32, 512); tmpcb = Buf()
    S.dma("sp", odT[0:8, :], c.ptail[1][0:8, :], writes=[odTb])
    S.dma("sp", RD[32:40, :], c.ptail[1][8:16, :], writes=[RBb])
    S.op("act", lambda e: e.activation(out=nea[:], in_=c.hpt[0:8, 2:3], func=AF.Exp), reads=[c.cb], writes=[neab])
    S.op("dve", lambda e: e.tensor_scalar(out=nea[:], in0=nea[:], scalar1=-1.0, scalar2=None, op0=ALU.mult), reads=[neab], writes=[neab])
    S.op("act", lambda e: e.activation(out=odT[0:8, :], in_=odT[0:8, :], func=AF.Exp, bias=c.hpt[0:8, 3:4]), reads=[odTb, c.cb], writes=[odTb])
    S.op("act", lambda e: e.activation(out=odT[0:8, :], in_=odT[0:8, :], func=AF.Ln, bias=c.epst[0:8, 2:3]), reads=[odTb, c.cb], writes=[odTb])
    S.op("dve", lambda e: e.tensor_scalar(out=odT[0:8, :], in0=odT[0:8, :], scalar1=nea[:], scalar2=None, op0=ALU.mult), reads=[odTb, neab], writes=[odTb])
    S.op("dve", lambda e: e.tensor_tensor_scan(out=RD[0:8, :], data0=c.cmask[0:8, :], data1=odT[0:8, :], initial=0.0, op0=ALU.mult, op1=ALU.add), reads=[odTb, c.cb], writes=[RDb])
    S.op("act", lambda e: e.activation(out=RD[32:40, :], in_=RD[32:40, :], func=AF.Sigmoid), reads=[RBb], writes=[RBb])
    for (p0, rb_, off) in ((32, RBb, 0), (0, RDb, 8)):
        for j in range(NT):
            S.op("pe", lambda e, j=j, p0=p0: e.transpose(out=c.ps[6][:, j * 8:(j + 1) * 8], in_=RD[p0:p0 + 8, j * 128:(j + 1) * 128], identity=c.identf[p0:p0 + 8, p0:p0 + 8]), reads=[rb_, c.cb], writes=[c.psb[6]], inc=(j == NT - 1))
        S.op("dve", lambda e, off=off: e.tensor_copy(out=cols[:, :, off:off + 8], in_=c.ps[6][:, 0:128].rearrange("p (j r) -> p j r", r=8)), reads=[c.psb[6]], writes=[colsb])
    S.op("act", lambda e: e.activation(out=tmpc[:], in_=cols[:, :, 8:16], func=AF.Exp), reads=[colsb], writes=[tmpcb])
    S.op("dve", lambda e: e.tensor_tensor(out=c3[:, :, :, 0], in0=tmpc[:], in1=cols[:, :, 0:8], op=ALU.mult), reads=[tmpcb, colsb], writes=[c3b])
    DR = al([128, S_LEN], F32, 8192); DRb = Buf()
    BR, BR_off = al.at([128, S_LEN], F32, 8192); BRb = Buf()
    ARG = c.sbt([128, G, 3, 128], F32, BR_off)
    GUm = al([128, 16, 128], BF16, 4096); GUmb = [Buf() for _ in range(NT // 4)]
    qT = al([128, S_LEN], BF16, 4096); qTb = Buf()
    kT = al([128, S_LEN], BF16, 4096); kTb = Buf()
    kTB, kTB_off = al.at([128, S_LEN], BF16, 4096); kTBb = Buf()
    zt = al([128, S_LEN], BF16, 4096); ztb = Buf()
    Kt = al([128, 16, 128], BF16, 4096); Ktb = Buf()
    Vt = al([128, 16, 128], BF16, 4096); Vtb = Buf()
    Xbb = [Buf() for _ in range(NGR)]
    xp0, xp0_off = al.at([128, 16 + S_LEN], BF16, 4128)
    xp1, xp1_off = al.at([128, 16 + S_LEN], BF16, 4128)
    Xb = al([128, 16, 128], BF16, 4096)
    xp = [xp0, xp1, xp0]; xpb = [Buf(), Buf()]; xpb.append(xpb[0])
    edq = c.sbt([128, S_LEN], BF16, xp0_off + 32)
    acc, acc_off = al.at([128, S_LEN], F32, 8192); accb = Buf()
    vb_all = al([128, 16, 128], BF16, 4096); vbab = Buf()
    kbe_all = al([128, 16, 128], BF16, 4096); kbeb = Buf()
    rn, rn_off = al.at([128, S_LEN], F32, 8192); rnb = Buf()
    kd_all = al([128, 16, 128], BF16, 4096); kdab = Buf()
    qd_all = al([128, S_LEN], BF16, 4096); qdab = Buf()
    sq, sq_off = al.at([128, S_LEN], BF16, 4096); sqb = Buf()
    nw_all = al([128, 16, 128], BF16, 4096)
    vT, vT_off = al.at([128, S_LEN], BF16, 4096); vTb = Buf()
    qk_all = al([128, 16, 128], BF16, 4096); qkb = [Buf() for _ in range(NT // 4)]
    sqf = [al([128, 512], BF16, 1024)] * 2; sqfb = [Buf()] * 2
    rnf = [al([128, 512], F32, 2048)] * 2; rnfb = [Buf()] * 2
    nwb = [Buf() for _ in range(NT // 4)]
    al.segs[0][0] = al.segs[0][1]
    ED = [al([128, G, 128], BF16, 1024) for _ in range(2)]; EDb = [Buf() for _ in range(2)]
    EA = [al([128, G, 3, 128], BF16, 3072) for _ in range(2)]; EAb = [Buf() for _ in range(2)]
    Nf = [al([128, G, 128], F32, 2048) for _ in range(2)]; Nfb = [Buf() for _ in range(2)]
    MM = [[al([128, 2, G, 128], BF16, 2048) for _ in range(2)] for _ in range(2)]; MMb = [[[Buf(), Buf()] for _ in range(2)] for _ in range(2)]
    R12 = [al([128, G, 2, 128], BF16, 2048) for _ in range(2)]; R12b = [Buf() for _ in range(2)]
    Pb2 = [[al([128, G, 128], BF16, 1024) for _ in range(2)] for _ in range(2)]; Pbb2 = [[Buf() for _ in range(2)] for _ in range(2)]
    PT = [al([128, G, 128], BF16, 1024) for _ in range(2)]; PTb = [Buf() for _ in range(2)]
    Yb = [al([128, G, 128], BF16, 1024) for _ in range(2)]; Ybb = [Buf() for _ in range(2)]
    vn_ = [al([128, 128], BF16, 256) for _ in range(2)]; vnb = [Buf() for _ in range(2)]
    Sf = al([128, 128], F32, 512); Sfb = Buf()
    Sb = [al([128, 128], BF16, 256) for _ in range(2)]; Sbb = [Buf() for _ in range(2)]
    dummy = al([128, 8], F32, 32); dummyb = Buf()
    acch = [Buf(), Buf()]; rnh = [Buf(), Buf()]; sqh = [Buf(), Buf()]
    for i in range(2):
        S.op("pool", lambda e, i=i: e.memset(xp[i][:, 0:16], 0.0), writes=[xpb[i]])
    eps_ap = c.epst[:, 0:1]
    DR3 = DR[:].rearrange("p (j t) -> p j t", t=128)
    identG = c.identf[:].unsqueeze(1).to_broadcast([128, G, 128])
    for h in range(8):
        S.dma("sp", xp[0][:, 16:16 + S_LEN], c.projT[1][16 + h], writes=[xpb[0]])
        S.dma("sp", xp[1][:, 16:16 + S_LEN], c.projT[1][24 + h], writes=[xpb[1]])
        S.dma("sp", zt[:], c.projT[1][40 + h], writes=[ztb])
        dlr = dlr2[h % 2]; dlrb = dlrb2[h % 2]
        rep_rows(c, DR, DRb, RD, RDb, h, 8, p0=0)
        rep_rows(c, BR, BRb, RD, RBb, h, 8, p0=32)
        S.op("act", lambda e, dlr=dlr: e.activation(out=dlr[:], in_=DR3[:, :, 127], func=AF.Exp), reads=[DRb], writes=[dlrb])
        S.op("dve", lambda e, h=h: e.tensor_tensor(out=tmpc[:, :, h], in0=DR3[:, :, 127], in1=cols[:, :, 8 + h], op=ALU.subtract), reads=[DRb, colsb], writes=[tmpcb])
        S.op("act", lambda e, h=h: e.activation(out=c3[:, :, h, 1], in_=tmpc[:, :, h], func=AF.Exp), reads=[tmpcb], writes=[c3b])
        fence_w = [accb, rnb, sqb] + acch + rnh + sqh
        S.op("pool", lambda e: e.memset(dummy[:], 0.0), writes=fence_w + [dummyb])
        for which, dstT, dstTb in ((0, qT, qTb), (1, kT, kTb), (2, vT, vTb)):
            if which == 2:
                S.dma("sp", xp[2][:, 16:16 + S_LEN], c.projT[1][32 + h], writes=[xpb[2]])
            for hf in range(2):
                c0, c1 = hf * 1024, (hf + 1) * 1024
                ab_, rb_, sb_ = acch[hf], rnh[hf], sqh[hf]
                conv4(c, acc, ab_, xp[which], xpb[which], PP_CDW + 4 * (8 * which + h), pad=16, c0=c0, c1=c1)
                if which == 2:
                    S.op("act", lambda e, c0=c0, c1=c1: e.activation(out=vT[:, c0:c1], in_=acc[:, c0:c1], func=AF.Silu), reads=[ab_], writes=[vTb])
                else:
                    S.op("act", lambda e, c0=c0, c1=c1: e.activation(out=acc[:, c0:c1], in_=acc[:, c0:c1], func=AF.Silu), reads=[ab_], writes=[ab_])
                    sumsq_rstd(c, rn, rb_, acc, ab_, sq, sb_, 1.0, eps_ap, c0=c0, c1=c1)
                    if which == 0:
                        S.op("dve", lambda e, dstT=dstT, c0=c0, c1=c1: e.scalar_tensor_tensor(out=dstT[:, c0:c1], in0=acc[:, c0:c1], scalar=float(128 ** -0.5), in1=rn[:, c0:c1], op0=ALU.mult, op1=ALU.mult), reads=[ab_, rb_], writes=[dstTb])
                    else:
                        S.op("dve", lambda e, dstT=dstT, c0=c0, c1=c1: e.tensor_tensor(out=dstT[:, c0:c1], in0=acc[:, c0:c1], in1=rn[:, c0:c1], op=ALU.mult), reads=[ab_, rb_], writes=[dstTb])
        S.op("pool", lambda e: e.memset(dummy[:], 0.0), writes=fence_w + [dummyb])
        S.op("pool", lambda e: e.tensor_tensor(out=kTB[:], in0=kT[:], in1=BR[:], op=ALU.mult), reads=[kTb, BRb], writes=[kTBb])
        S.op("act", lambda e: e.activation(out=edq[:], in_=DR[:], func=AF.Exp), reads=[DRb, xpb[0]], writes=[xpb[0]])
        S.op("pool", lambda e: e.tensor_tensor(out=qd_all[:], in0=qT[:], in1=edq[:], op=ALU.mult), reads=[qTb, xpb[0]], writes=[qdab])
        to_tokmajor(c, Kt, Ktb, kT, kTb)
        to_tokmajor(c, Vt, Vtb, vT, vTb)
        bc = lambda ap: ap.to_broadcast([128, 16, 128])
        S.op("dve", lambda e, h=h: e.tensor_tensor(out=vb_all[:], in0=Vt[:], in1=bc(cols[:, :, h:h + 1]), op=ALU.mult), reads=[Vtb, colsb], writes=[vbab])
        S.op("pool", lambda e, h=h: e.tensor_tensor(out=kbe_all[:], in0=Kt[:], in1=bc(c3[:, :, h, 0:1]), op=ALU.mult), reads=[Ktb, c3b], writes=[kbeb])
        S.op("dve", lambda e, h=h: e.tensor_tensor(out=kd_all[:], in0=Kt[:], in1=bc(c3[:, :, h, 1:2]), op=ALU.mult), reads=[Ktb, c3b], writes=[kdab])
        for gi in range(NGR):
            k = gi % 2
            b0, b1, b2, b3 = 4 * k, 4 * k + 1, 4 * k + 2, 4 * k + 3
            blks = range(G * gi, G * gi + G)
            pv3 = c.ps[b3][:].bitcast(BF16)
            for bi, n in enumerate(blks):
                tsl = slice(n * 128, (n + 1) * 128)
                dcol = cols[:, n, 8 + h:9 + h]
                S.op("dve", lambda e, b0=b0, b1=b1, b2=b2, b3=b3, pv3=pv3, bi=bi, tsl=tsl, dcol=dcol: e.scalar_tensor_tensor(out=ARG[:, bi, 0:2, :], in0=DR[:, tsl].unsqueeze(1).to_broadcast([128, 2, 128]), scalar=dcol, in1=c.negu2[:], op0=ALU.subtract, op1=ALU.min),
                     reads=[DRb, colsb, BRb, c.cb], writes=[BRb])
            S.op("act", lambda e, b0=b0, b1=b1, b2=b2, b3=b3, pv3=pv3, gi=gi: e.activation(out=GUm[:, G * gi:G * gi + G, :], in_=ARG[:, :, 0, :], func=AF.Exp), reads=[BRb], writes=[GUmb[gi]])
            S.op("act", lambda e, b0=b0, b1=b1, b2=b2, b3=b3, pv3=pv3, k=k: e.activation(out=ED[k][:], in_=ARG[:, :, 1, :], func=AF.Exp), reads=[BRb], writes=[EDb[k]])
            for bi, n in enumerate(blks):
                tsl = slice(n * 128, (n + 1) * 128)
                dcol = cols[:, n, 8 + h:9 + h]
                S.op("dve", lambda e, b0=b0, b1=b1, b2=b2, b3=b3, pv3=pv3, bi=bi, tsl=tsl, dcol=dcol: e.scalar_tensor_tensor(out=ARG[:, bi, :, :], in0=DR[:, tsl].unsqueeze(1).to_broadcast([128, 3, 128]), scalar=dcol, in1=c.posl3[:], op0=ALU.subtract, op1=ALU.max),
                     reads=[DRb, colsb, BRb, c.cb], writes=[BRb])
            S.op("act", lambda e, b0=b0, b1=b1, b2=b2, b3=b3, pv3=pv3, k=k: e.activation(out=EA[k][:], in_=ARG[:], func=AF.Exp, scale=-1.0), reads=[BRb], writes=[EAb[k]])
            for bi, n in enumerate(blks):
                tsl = slice(n * 128, (n + 1) * 128)
                S.op("pe", lambda e, b0=b0, b1=b1, b2=b2, b3=b3, pv3=pv3, bi=bi, tsl=tsl: e.matmul(c.ps[b0][:, bi * 128:(bi + 1) * 128], lhsT=kT[:, tsl], rhs=kTB[:, tsl], start=True, stop=True), reads=[kTb, kTBb], writes=[c.psb[b0]], inc=(bi == G - 1))
            for bi, n in enumerate(blks):
                tsl = slice(n * 128, (n + 1) * 128)
                S.op("pe", lambda e, b0=b0, b1=b1, b2=b2, b3=b3, pv3=pv3, bi=bi, tsl=tsl: e.matmul(c.ps[b1][:, bi * 128:(bi + 1) * 128], lhsT=kTB[:, tsl], rhs=kT[:, tsl], start=True, stop=True), reads=[kTb, kTBb], writes=[c.psb[b1]], inc=(bi == G - 1))
            pv0 = c.ps[b0][:, :].rearrange("p (g t) -> p g t", t=128)
            pv1 = c.ps[b1][:, :].rearrange("p (g t) -> p g t", t=128)
            pv2 = c.ps[b2][:, :].rearrange("p (g t) -> p g t", t=128)
            S.op("dve", lambda e, b0=b0, b1=b1, b2=b2, b3=b3, pv3=pv3, k=k, pv0=pv0: e.scalar_tensor_tensor(out=Nf[k][:], in0=pv0, scalar=-1.0, in1=ED[k][:], op0=ALU.mult, op1=ALU.mult), reads=[c.psb[b0], EDb[k]], writes=[Nfb[k]])
            S.op("act", lambda e, b0=b0, b1=b1, b2=b2, b3=b3, pv3=pv3, k=k: e.activation(out=MM[k][0][:, 0], in_=Nf[k][:], func=AF.Copy), reads=[Nfb[k]], writes=[MMb[k][0][0]])
            S.op("dve", lambda e, b0=b0, b1=b1, b2=b2, b3=b3, pv3=pv3, k=k, pv1=pv1: e.scalar_tensor_tensor(out=MM[k][0][:, 1], in0=pv1, scalar=-1.0, in1=EA[k][:, :, 0, :], op0=ALU.mult, op1=ALU.mult), reads=[c.psb[b1], EAb[k]], writes=[MMb[k][0][1]])
            S.op("dve", lambda e, b0=b0, b1=b1, b2=b2, b3=b3, pv3=pv3, k=k, pv1=pv1: e.tensor_tensor(out=R12[k][:], in0=pv1.unsqueeze(2).to_broadcast([128, G, 2, 128]), in1=EA[k][:, :, 1:3, :], op=ALU.mult), reads=[c.psb[b1], EAb[k]], writes=[R12b[k]])
            pc = 0
            S.op("pool", lambda e, k=k: e.tensor_tensor(out=Pb2[k][0][:], in0=Nf[k][:], in1=identG, op=ALU.add), reads=[Nfb[k], c.cb], writes=[Pbb2[k][0]])
            cur = 0
            for lvl in range(4):
                nxt = 1 - cur
                last = (lvl == 3)
                for bi in range(G):
                    S.op("pe", lambda e, b1=b1, bi=bi, k=k, cur=cur: e.matmul(c.ps[b1][:, bi * 128:(bi + 1) * 128], lhsT=MM[k][cur][:, 0, bi, :], rhs=MM[k][cur][:, 1, bi, :], start=True, stop=True),
                         reads=MMb[k][cur], writes=[c.psb[b1]], inc=(bi == G - 1))
                if not last:
                    for bi in range(G):
                        S.op("pe", lambda e, b0=b0, bi=bi, k=k, cur=cur: e.matmul(c.ps[b0][:, bi * 128:(bi + 1) * 128], lhsT=MM[k][cur][:, 1, bi, :], rhs=MM[k][cur][:, 0, bi, :], start=True, stop=True),
                             reads=MMb[k][cur], writes=[c.psb[b0]], inc=(bi == G - 1))
                S.op("act", lambda e, k=k, nxt=nxt, pv1=pv1: e.activation(out=MM[k][nxt][:, 1], in_=pv1, func=AF.Copy), reads=[c.psb[b1]], writes=[MMb[k][nxt][1]])
                if not last:
                    S.op("act", lambda e, k=k, nxt=nxt, pv0=pv0: e.activation(out=MM[k][nxt][:, 0], in_=pv0, func=AF.Copy), reads=[c.psb[b0]], writes=[MMb[k][nxt][0]])
                for bi in range(G):
                    S.op("pe", lambda e, b2=b2, bi=bi, k=k, nxt=nxt, pc=pc: e.matmul(c.ps[b2][:, bi * 128:(bi + 1) * 128], lhsT=MM[k][nxt][:, 1, bi, :], rhs=Pb2[k][pc][:, bi, :], start=True, stop=False),
                         reads=[MMb[k][nxt][1], Pbb2[k][pc]], writes=[c.psb[b2]], inc=False)
                    S.op("pe", lambda e, b2=b2, bi=bi, k=k, pc=pc: e.matmul(c.ps[b2][:, bi * 128:(bi + 1) * 128], lhsT=c.ident[:], rhs=Pb2[k][pc][:, bi, :], start=False, stop=True),
                         reads=[c.cb, Pbb2[k][pc]], writes=[c.psb[b2]], inc=(bi == G - 1))
                S.op("act", lambda e, k=k, pv2=pv2, pc=pc: e.activation(out=Pb2[k][1 - pc][:], in_=pv2, func=AF.Copy), reads=[c.psb[b2]], writes=[Pbb2[k][1 - pc]])
                pc = 1 - pc
                cur = nxt
            for r in range(2):
                for bi in range(G):
                    S.op("pe", lambda e, pv3=pv3, bi=bi, k=k, pc=pc: e.transpose(out=pv3[:, bi * 128:(bi + 1) * 128], in_=Pb2[k][pc][:, bi, :], identity=c.ident[:]), reads=[Pbb2[k][pc], c.cb], writes=[c.psb[b3]], inc=(bi == G - 1))
                S.op("act", lambda e, pv3=pv3, k=k: e.activation(out=PT[k][:], in_=pv3[:, 0:G * 128].rearrange("p (g t) -> p g t", t=128), func=AF.Copy), reads=[c.psb[b3]], writes=[PTb[k]])
                for bi in range(G):
                    S.op("pe", lambda e, b1=b1, bi=bi, k=k, r=r, pc=pc: e.matmul(c.ps[b1][:, bi * 128:(bi + 1) * 128], lhsT=R12[k][:, bi, r, :], rhs=Pb2[k][pc][:, bi, :], start=True, stop=True), reads=[R12b[k], Pbb2[k][pc]], writes=[c.psb[b1]], inc=(bi == G - 1))
                S.op("act", lambda e, k=k, pv1=pv1: e.activation(out=Yb[k][:], in_=pv1, func=AF.Copy, scale=-1.0), reads=[c.psb[b1]], writes=[Ybb[k]])
                for bi in range(G):
                    S.op("pe", lambda e, b0=b0, bi=bi, k=k, pc=pc: e.matmul(c.ps[b0][:, bi * 128:(bi + 1) * 128], lhsT=c.ident[:], rhs=Pb2[k][pc][:, bi, :], start=True, stop=False), reads=[c.cb, Pbb2[k][pc]], writes=[c.psb[b0]], inc=False)
                    S.op("pe", lambda e, b0=b0, bi=bi, k=k: e.matmul(c.ps[b0][:, bi * 128:(bi + 1) * 128], lhsT=PT[k][:, bi, :], rhs=Yb[k][:, bi, :], start=False, stop=True), reads=[PTb[k], Ybb[k]], writes=[c.psb[b0]], inc=(bi == G - 1))
                if r == 1:
                    S.op("act", lambda e, k=k, gi=gi, pv0=pv0: e.activation(out=Xb[:, G * gi:G * gi + G, :], in_=pv0, func=AF.Copy), reads=[c.psb[b0]], writes=[Xbb[gi]])
                else:
                    S.op("act", lambda e, k=k, pv0=pv0, pc=pc: e.activation(out=Pb2[k][1 - pc][:], in_=pv0, func=AF.Copy), reads=[c.psb[b0]], writes=[Pbb2[k][1 - pc]])
                    pc = 1 - pc
            for bi, n in enumerate(blks):
                tsl = slice(n * 128, (n + 1) * 128)
                S.op("pe", lambda e, b0=b0, b1=b1, b2=b2, b3=b3, pv3=pv3, bi=bi, tsl=tsl: e.matmul(c.ps[b1][:, bi * 128:(bi + 1) * 128], lhsT=kT[:, tsl], rhs=qT[:, tsl], start=True, stop=True), reads=[kTb, qTb], writes=[c.psb[b1]], inc=(bi == G - 1))
            S.op("dve", lambda e, b0=b0, b1=b1, b2=b2, b3=b3, pv3=pv3, gi=gi, pv1=pv1: e.tensor_tensor(out=qk_all[:, G * gi:G * gi + G, :], in0=pv1, in1=GUm[:, G * gi:G * gi + G, :], op=ALU.mult), reads=[c.psb[b1], GUmb[gi]], writes=[qkb[gi]])
            for bi, n in enumerate(blks):
                S.op("pe", lambda e, b0=b0, b1=b1, b2=b2, b3=b3, pv3=pv3, bi=bi, n=n: e.matmul(c.ps[b2][:, bi * 128:(bi + 1) * 128], lhsT=kbe_all[:, n, :], rhs=Xb[:, n, :], start=True, stop=True), reads=[kbeb, Xbb[gi]], writes=[c.psb[b2]], inc=(bi == G - 1))
            S.op("act", lambda e, b0=b0, b1=b1, b2=b2, b3=b3, pv3=pv3, gi=gi, pv2=pv2: e.activation(out=nw_all[:, G * gi:G * gi + G, :], in_=pv2, func=AF.Copy, scale=-1.0), reads=[c.psb[b2]], writes=[nwb[gi]])
        S.op("pool", lambda e: e.memset(Sf[:], 0.0), writes=[Sfb])
        S.op("pool", lambda e: e.memset(Sb[0][:], 0.0), writes=[Sbb[0]])
        for n in range(NT):
            k = n % 2
            gi = n // G
            tsl = slice(n * 128, (n + 1) * 128)
            bkB, bkC, bkD = 0 + k, 2 + k, 4 + k
            sc_, sn_ = Sb[k], Sb[1 - k]
            scb_, snb_ = Sbb[k], Sbb[1 - k]
            S.op("pe", lambda e, bkB=bkB, n=n: e.matmul(c.ps[bkB][:, 0:128], lhsT=Xb[:, n, :], rhs=vb_all[:, n, :], start=True, stop=False), reads=[Xbb[gi], vbab], writes=[c.psb[bkB]], inc=False)
            S.op("pe", lambda e, bkB=bkB, n=n, sc_=sc_: e.matmul(c.ps[bkB][:, 0:128], lhsT=nw_all[:, n, :], rhs=sc_[:], start=False, stop=True), reads=[nwb[gi], scb_], writes=[c.psb[bkB]])
            S.op("act", lambda e, bkB=bkB, k=k: e.activation(out=vn_[k][:], in_=c.ps[bkB][:, 0:128], func=AF.Copy), reads=[c.psb[bkB]], writes=[vnb[k]])
            if n + 1 < NT:
                S.op("pe", lambda e, bkD=bkD, k=k, n=n: e.matmul(c.ps[bkD][:, 0:128], lhsT=kd_all[:, n, :], rhs=vn_[k][:], start=True, stop=True), reads=[kdab, vnb[k]], writes=[c.psb[bkD]])
                S.op("dve", lambda e, bkD=bkD, n=n, dlr=dlr: e.scalar_tensor_tensor(out=Sf[:], in0=Sf[:], scalar=dlr[:, n:n + 1], in1=c.ps[bkD][:, 0:128], op0=ALU.mult, op1=ALU.add), reads=[c.psb[bkD], Sfb, dlrb], writes=[Sfb])
                S.op("act", lambda e, sn_=sn_: e.activation(out=sn_[:], in_=Sf[:], func=AF.Copy), reads=[Sfb], writes=[snb_])
            S.op("pe", lambda e, bkC=bkC, tsl=tsl, sc_=sc_: e.matmul(c.ps[bkC][:, 0:128], lhsT=sc_[:], rhs=qd_all[:, tsl], start=True, stop=False), reads=[scb_, qdab], writes=[c.psb[bkC]], inc=False)
            S.op("pe", lambda e, bkC=bkC, k=k, n=n: e.matmul(c.ps[bkC][:, 0:128], lhsT=vn_[k][:], rhs=qk_all[:, n, :], start=False, stop=True), reads=[vnb[k], qkb[gi]], writes=[c.psb[bkC]])
            S.op("dve", lambda e, bkC=bkC, tsl=tsl: e.tensor_copy(out=odT[:, tsl], in_=c.ps[bkC][:, 0:128]), reads=[c.psb[bkC]], writes=[odTb])
        S.op("act", lambda e: e.activation(out=zt[:], in_=zt[:], func=AF.Silu), reads=[ztb], writes=[ztb])
        for nb in range(4):
            kk = nb % 2
            cs = slice(nb * 512, (nb + 1) * 512)
            bk = 6 + kk
            S.op("pool", lambda e, kk=kk, cs=cs: e.tensor_tensor(out=sqf[kk][:], in0=odT[:, cs], in1=odT[:, cs], op=ALU.mult), reads=[odTb], writes=[sqfb[kk]])
            S.op("pe", lambda e, kk=kk, bk=bk: e.matmul(c.ps[bk][:, :], lhsT=c.ones[:], rhs=sqf[kk][:], start=True, stop=True), reads=[sqfb[kk], c.cb], writes=[c.psb[bk]])
            S.op("act", lambda e, kk=kk, bk=bk: e.activation(out=rnf[kk][:], in_=c.ps[bk][:, :], func=AF.Ln, scale=1.0 / 128, bias=eps_ap), reads=[c.psb[bk], c.cb], writes=[rnfb[kk]])
            S.op("act", lambda e, kk=kk: e.activation(out=rnf[kk][:], in_=rnf[kk][:], func=AF.Exp, scale=-0.5), reads=[rnfb[kk]], writes=[rnfb[kk]])
            S.op("dve", lambda e, kk=kk, cs=cs: e.scalar_tensor_tensor(out=rnf[kk][:], in0=odT[:, cs], scalar=c.ppt[:, PP_ON:PP_ON + 1], in1=rnf[kk][:], op0=ALU.mult, op1=ALU.mult), reads=[odTb, rnfb[kk], c.cb], writes=[rnfb[kk]])
            S.op("pool", lambda e, kk=kk, cs=cs: e.tensor_tensor(out=zt[:, cs], in0=rnf[kk][:], in1=zt[:, cs], op=ALU.mult), reads=[rnfb[kk], ztb], writes=[ztb])
        S.dma("sp", c.ydscr[h], zt[:], reads=[ztb], writes=[c.ydb[h]])


def host_layout(inputs):
    f = np.float32
    ev_w_in = inputs["ev_w_in"][0]
    od_w_in = inputs["od_w_in"][0]
    com = {}
    com["w_in0"] = np.ascontiguousarray(ev_w_in[:, :9216].reshape(16, 128, 72, 128).transpose(2, 1, 0, 3))
    com["w_tail0"] = np.ascontiguousarray(ev_w_in[:, 9216:9224].reshape(16, 128, 8).transpose(1, 0, 2))
    com["w_in1"] = np.ascontiguousarray(od_w_in[:, :6144].reshape(16, 128, 48, 128).transpose(2, 1, 0, 3))
    com["w_tail1"] = np.ascontiguousarray(od_w_in[:, 6144:6160].reshape(16, 128, 16).transpose(1, 0, 2))
    com["w_out0"] = np.ascontiguousarray(inputs["ev_w_out"][0].reshape(16, 128, 4, 512).transpose(2, 1, 0, 3))
    com["w_out1"] = np.ascontiguousarray(inputs["od_w_out"][0].reshape(16, 128, 4, 512).transpose(2, 1, 0, 3))
    com["gate_w"] = np.ascontiguousarray(inputs["od_gate_w"][0])
    pp = np.zeros((128, PP_N), f)
    pp[:, PP_EVN:PP_EVN + 16] = inputs["ev_norm"][0].reshape(16, 128).T
    pp[:, PP_ODN:PP_ODN + 16] = inputs["od_norm"][0].reshape(16, 128).T
    pp[:, PP_QN] = inputs["ev_qn_gain"][0]
    pp[:, PP_KN] = inputs["ev_kn_gain"][0]
    pp[:, PP_ON] = inputs["od_onorm"][0]
    ccw = inputs["od_conv_c_w"][0]
    pp[:, PP_CCW:PP_CCW + 32] = ccw.reshape(4, 8, 128).transpose(2, 1, 0).reshape(128, 32)
    pp[:, PP_CCB:PP_CCB + 8] = inputs["od_conv_c_b"][0].reshape(8, 128).T
    gbias = inputs["od_gate_b"][0]
    pp[:, PP_GBR:PP_GBR + 8] = gbias[:1024].reshape(8, 128).T
    pp[:, PP_GBI:PP_GBI + 8] = gbias[1024:].reshape(8, 128).T
    pp[:, PP_LAM:PP_LAM + 8] = inputs["od_lambda"][0].reshape(8, 128).T
    cdw = inputs["od_conv_d_w"][0]
    pp[:, PP_CDW:PP_CDW + 96] = cdw.reshape(4, 24, 128).transpose(2, 1, 0).reshape(128, 96)
    com["pp"] = pp
    hp = np.zeros((8, 4), f)
    hp[0:4, 0] = inputs["ev_if_bias"][0][0:4]
    hp[0:4, 1] = inputs["ev_if_bias"][0][4:8]
    hp[:, 2] = inputs["od_a_log"][0]
    hp[:, 3] = inputs["od_dt_bias"][0]
    com["hp"] = hp
    rb = inputs["ev_rel_bias"][0]
    kl = np.arange(128)[:, None, None]
    jb = np.arange(5)[None, :, None]
    i = np.arange(128)[None, None, :]
    j = jb * 128 + kl
    rel = np.clip(512 + i - j, -256, 256) + 256
    valid = ((i // 64) <= (j // 64)) & ((j // 64) <= 8 + (i // 64))
    tab = rb[:, rel]
    tab = np.where(valid[None], tab, f(NEG)).astype(f)
    com["bt"] = np.ascontiguousarray(tab.reshape(8, 128, 640))
    return com


_CACHE = {}


def kernel(**inputs):
    x = np.ascontiguousarray(inputs["x"], dtype=np.float32)
    com = host_layout(inputs)
    if "nc" not in _CACHE:
        _CACHE["nc"] = build()[0]
    nc = _CACHE["nc"]
    in_maps = []
    for b in range(8):
        m = dict(com)
        m["x"] = x[b]
        in_maps.append(m)
    res = run_bass_kernel_spmd(nc, in_maps, core_ids=list(range(8)))
    out = np.stack([np.asarray(r["out"], dtype=np.float32) for r in res.results], axis=0)
    return out
```

```python
import numpy as np
from contextlib import ExitStack
import concourse.bass as bass
import concourse.mybir as mybir
from concourse.bass_utils import run_bass_kernel_spmd

F32 = mybir.dt.float32
BF16 = mybir.dt.bfloat16
AF = mybir.ActivationFunctionType
ALU = mybir.AluOpType
AX = mybir.AxisListType

S_LEN = 2048
D = 2048
NT = 16
EPS = 1e-6
NEG = -1e30


class Buf:
    __slots__ = ("name", "w", "r")

    def __init__(self, name=""):
        self.name = name
        self.w = None
        self.r = {}


class _FakeInst:
    def then_inc(self, *a, **k):
        return self


class _FakeEng:
    def __init__(self):
        self.calls = []

    def __getattr__(self, name):
        def f(*a, **k):
            self.calls.append((name, a, k))
            return _FakeInst()
        return f


def _free_elems(ap):
    sh = ap.shape
    n = 1
    for d in sh[1:]:
        n *= int(d)
    return n


def _est_cost(eng, fns):
    fk = _FakeEng()
    for f in fns:
        f(fk)
    tot = 0.0
    for (name, a, k) in fk.calls:
        if name == "matmul":
            rhs = k.get("rhs")
            n = _free_elems(rhs)
            mult = 4.0 if rhs.dtype == F32 else 1.0
            tot += mult * max(64, n) * 0.45 + 15
        elif name == "transpose":
            tot += 128 * 0.45 + 30
        else:
            out = k.get("out", a[0] if a else None)
            n = _free_elems(out) if out is not None else 64
            if name == "tensor_tensor_scan":
                n *= 2
            if eng == "pool":
                tot += 350 + 2.0 * n
            elif eng == "act":
                tot += 220 + 1.05 * n
            else:
                tot += 200 + 1.05 * n
    return tot


class Sched:
    ENG = ("pe", "act", "dve", "pool", "sp")
    DMA_POOLS = {"sp": 10, "act": 4, "pool": 8, "pe": 1, "dve": 1}

    def __init__(self, nc, same_engine_wait=True, reorder=True):
        self.nc = nc
        self.same_engine_wait = same_engine_wait
        self.reorder = reorder
        self.nodes = []
        self.segments = []
        self.pending = {e: None for e in self.ENG}
        self.sems = {}
        self.n_ops = 0
        self.n_waits = 0
        self.q = {e: [] for e in self.ENG}
        self.dma_keys = {}
        idx = 0
        for e, n in self.DMA_POOLS.items():
            self.dma_keys[e] = [("dma", idx + t) for t in range(n)]
            idx += n
        self.N_DMA_SEM = idx

    def op(self, eng, fn, reads=(), writes=(), inc=True):
        self.n_ops += 1
        p = self.pending[eng]
        if p is None:
            p = {"eng": eng, "kind": "op", "fns": [], "reads": [], "writes": []}
        p["fns"].append(fn)
        p["reads"].extend(reads)
        p["writes"].extend(writes)
        if inc:
            self.pending[eng] = None
            self.nodes.append(p)
        else:
            self.pending[eng] = p

    def dma(self, eng, out_ap, in_ap, reads=(), writes=(), **kw):
        assert self.pending[eng] is None
        self.n_ops += 1
        self.nodes.append({"eng": eng, "kind": "dma", "out": out_ap, "in": in_ap, "kw": kw, "reads": list(reads), "writes": list(writes)})

    def barrier(self):
        for e in self.ENG:
            assert self.pending[e] is None, e
        self.segments.append(self.nodes)
        self.nodes = []

    def _schedule(self, nodes):
        n = len(nodes)
        preds = [set() for _ in range(n)]
        lastw = {}
        readers = {}
        for i, nd in enumerate(nodes):
            for b in nd["reads"]:
                w = lastw.get(id(b))
                if w is not None:
                    preds[i].add(w)
            for b in nd["writes"]:
                w = lastw.get(id(b))
                if w is not None:
                    preds[i].add(w)
                for r in readers.get(id(b), ()):
                    preds[i].add(r)
            for b in nd["writes"]:
                lastw[id(b)] = i
                readers[id(b)] = []
            for b in nd["reads"]:
                readers.setdefault(id(b), []).append(i)
            preds[i].discard(i)
        if not self.reorder:
            order = {e: [i for i in range(n) if nodes[i]["eng"] == e] for e in self.ENG}
            return preds, order
        cost = []
        for nd in nodes:
            if nd["kind"] == "dma":
                o = nd["out"]
                nbytes = _free_elems(o) * int(o.shape[0]) * (2 if o.dtype == BF16 else 4)
                nd["lat"] = 2200.0 + nbytes / 120.0
                cost.append(900.0 if nd["eng"] == "pool" else 120.0)
            else:
                cost.append(_est_cost(nd["eng"], nd["fns"]))
        succs = [[] for _ in range(n)]
        npred = [len(p) for p in preds]
        for i, p in enumerate(preds):
            for j in p:
                succs[j].append(i)
        done_t = [0.0] * n
        ready_t = [0.0] * n
        avail = {e: [] for e in self.ENG}
        for i in range(n):
            if npred[i] == 0:
                avail[nodes[i]["eng"]].append(i)
        free = {e: 0.0 for e in self.ENG}
        order = {e: [] for e in self.ENG}
        left = n
        WIN = 4000
        low = {e: 0 for e in self.ENG}
        while left:
            best = None
            for e in self.ENG:
                av = avail[e]
                if not av:
                    continue
                fe = free[e]
                m = min(av)
                bi = None
                bk = None
                for i in av:
                    if i > m + WIN:
                        continue
                    key = (max(ready_t[i], fe), i)
                    if bk is None or key < bk:
                        bk = key
                        bi = i
                if best is None or bk < best[0]:
                    best = (bk, e, bi)
            (st, _), e, i = best
            avail[e].remove(i)
            order[e].append(i)
            end = st + cost[i]
            free[e] = end
            done_t[i] = end if nodes[i]["kind"] != "dma" else st + nodes[i]["lat"]
            left -= 1
            for sidx in succs[i]:
                npred[sidx] -= 1
                if done_t[i] > ready_t[sidx]:
                    ready_t[sidx] = done_t[i]
                if npred[sidx] == 0:
                    avail[nodes[sidx]["eng"]].append(sidx)
        return preds, order

    def emit(self, block):
        if self.nodes:
            self.barrier()
        sems = self.sems
        cnt = {e: 0 for e in self.ENG}
        dma_tot = {}
        dma_rr = {e: 0 for e in self.ENG}
        seen = {e: {} for e in self.ENG}
        q = self.q

        def need(eng, evs):
            sn = seen[eng]
            best = {}
            for (k, v) in evs:
                if k == eng and (eng == "pe" or not self.same_engine_wait):
                    continue
                if sn.get(k, 0) >= v:
                    continue
                if best.get(k, 0) < v:
                    best[k] = v
            for k, v in best.items():
                sn[k] = v
                q[eng].append(("w", k, v))
                self.n_waits += 1

        for nodes in self.segments:
            preds, order = self._schedule(nodes)
            ev = [None] * len(nodes)
            for e in self.ENG:
                for i in order[e]:
                    nd = nodes[i]
                    if nd["kind"] == "op":
                        cnt[e] += 1
                        ev[i] = (e, cnt[e])
                    else:
                        keys = self.dma_keys[e]
                        key = keys[dma_rr[e] % len(keys)]
                        dma_rr[e] += 1
                        prev = dma_tot.get(key, 0)
                        nd["prev"] = (key, prev) if prev else None
                        dma_tot[key] = prev + 16
                        nd["key"] = key
                        ev[i] = (key, prev + 16)
            for e in self.ENG:
                for i in order[e]:
                    nd = nodes[i]
                    evs = [ev[j] for j in preds[i]]
                    if nd["kind"] == "dma" and nd["prev"] is not None:
                        evs.append(nd["prev"])
                    need(e, evs)
                    if nd["kind"] == "op":
                        q[e].append(("g", nd["fns"]))
                    else:
                        q[e].append(("d", nd["out"], nd["in"], nd["key"], nd["kw"]))
            allev = [(e, cnt[e]) for e in self.ENG if cnt[e] > 0] + [(k, t) for k, t in dma_tot.items()]
            for e in self.ENG:
                need(e, allev)

        def run(eng_name, e):
            for it in q[eng_name]:
                t = it[0]
                if t == "w":
                    e.wait_ge(sems[it[1]], it[2])
                elif t == "g":
                    fns = it[1]
                    for f in fns[:-1]:
                        f(e)
                    fns[-1](e).then_inc(sems[eng_name], 1)
                elif t == "d":
                    e.dma_start(out=it[1], in_=it[2], **it[4]).then_inc(sems[it[3]], 16)

        @block.tensor
        def _(e):
            run("pe", e)

        @block.scalar
        def _(e):
            run("act", e)

        @block.vector
        def _(e):
            run("dve", e)

        @block.gpsimd
        def _(e):
            run("pool", e)

        @block.sync
        def _(e):
            run("sp", e)


PP_EVN = 0
PP_ODN = 16
PP_QN = 32
PP_KN = 33
PP_ON = 34
PP_CCW = 35
PP_CCB = 67
PP_GBR = 75
PP_GBI = 83
PP_LAM = 91
PP_CDW = 99
PP_N = 195
SBUF_BASE = 16544
NS_MAX = 1000
SBUF_BYTES = 212832


class Ctx:
    pass


class Arena:
    def __init__(self, c, segs=None):
        self.c = c
        self.segs = [list(x) for x in (segs or [(0, 65536), (c.ARENA, SBUF_BYTES)])]

    def __call__(self, shape, dt, nbytes):
        n = _r32(nbytes)
        for sg in self.segs:
            if sg[0] + n <= sg[1]:
                t = self.c.sbt(shape, dt, sg[0])
                sg[0] += n
                return t
        raise AssertionError("SBUF arena overflow %s %d %s" % (shape, nbytes, self.segs))

    def at(self, shape, dt, nbytes):
        n = _r32(nbytes)
        for sg in self.segs:
            if sg[0] + n <= sg[1]:
                off = sg[0]
                t = self.c.sbt(shape, dt, off)
                sg[0] += n
                return t, off
        raise AssertionError("SBUF arena overflow %s %d %s" % (shape, nbytes, self.segs))


def _r32(n):
    return (int(n) + 31) // 32 * 32


def build(phases=("all",), dbg=False):
    nc = bass.Bass("TRN2", target_bir_lowering=False)
    allp = "all" in phases

    def on(p):
        return allp or p in phases

    def dram(name, shape, dt, kind=None):
        if kind is None:
            return nc.dram_tensor(name, shape, dt).ap()
        return nc.dram_tensor(name, shape, dt, kind=kind).ap()

    c = Ctx()
    c.nc = nc
    c.x = dram("x", [S_LEN, D], F32, "ExternalInput")
    c.win = [dram("w_in0", [72, 128, 16, 128], F32, "ExternalInput"), dram("w_in1", [48, 128, 16, 128], F32, "ExternalInput")]
    c.wtail = [dram("w_tail0", [128, 16, 8], F32, "ExternalInput"), dram("w_tail1", [128, 16, 16], F32, "ExternalInput")]
    c.wout = [dram("w_out0", [4, 128, 16, 512], F32, "ExternalInput"), dram("w_out1", [4, 128, 16, 512], F32, "ExternalInput")]
    c.gatew = dram("gate_w", [8, 128, 256], F32, "ExternalInput")
    c.pp = dram("pp", [128, PP_N], F32, "ExternalInput")
    c.hp = dram("hp", [8, 4], F32, "ExternalInput")
    c.bt = dram("bt", [8, 128, 640], F32, "ExternalInput")
    c.out = dram("out", [S_LEN, D], F32, "ExternalOutput")
    sk = lambda name, prod: ("ExternalOutput" if on(prod) else "ExternalInput") if dbg else None
    c.projT = [dram("projT0", [72, 128, S_LEN], BF16, sk("projT0", "gemm0")), dram("projT1", [48, 128, S_LEN], BF16, sk("projT1", "gemm1"))]
    c.ptail = [dram("ptail0", [8, S_LEN], F32, sk("ptail0", "gemm0")), dram("ptail1", [16, S_LEN], F32, sk("ptail1", "gemm1"))]
    c.ydscr = dram("ydscr", [8, 128, S_LEN], BF16)
    c.ydb = [Buf() for _ in range(8)]
    if dbg:
        c.ytd = dram("ytd", [16, 128, S_LEN], BF16, "ExternalOutput")
        c.xtd = dram("xtd", [16, 128, S_LEN], BF16, "ExternalOutput")
        c.ytin = dram("ytin", [16, 128, S_LEN], BF16, "ExternalInput")
        c.x1in = dram("x1in", [S_LEN, D], F32, "ExternalInput")
        c.dbgo = dram("dbgo", [10, 128, S_LEN], F32, "ExternalOutput")
    c.dbg = dbg

    with ExitStack() as es:
        S = Sched(nc)
        c.S = S
        sems = {}
        for e in S.ENG:
            sems[e] = es.enter_context(nc.semaphore("s_" + e))
        for i in range(S.N_DMA_SEM):
            sems[("dma", i)] = es.enter_context(nc.semaphore("d%d" % i))
        S.sems = sems
        c.uid = 0

        def sbt(shape, dt, off, name=None):
            c.uid += 1
            assert off % 32 == 0 and off + 1 <= SBUF_BYTES, off
            return nc.alloc_sbuf_tensor_at(name or ("t%d" % c.uid), shape, dt, offset=SBUF_BASE + off)
        c.sbt = sbt
        c.ps = [es.enter_context(nc.psum_tensor("ps%d" % i, [128, 512], F32)) for i in range(8)]
        c.psb = [Buf("ps%d" % i) for i in range(8)]
        block = es.enter_context(nc.Block())

        c.XT = sbt([128, 16, S_LEN], BF16, 0, "XT")
        c.YT = sbt([128, 16, S_LEN], BF16, 65536, "YT")
        c.xtb = [Buf("xt%d" % i) for i in range(16)]
        c.ytb = [Buf("yt%d" % i) for i in range(16)]
        CO = 131072
        c.ident = sbt([128, 128], BF16, CO, "ident"); CO += _r32(256)
        c.identf = sbt([128, 128], F32, CO, "identf"); CO += _r32(512)
        c.ones = sbt([128, 128], BF16, CO, "ones"); CO += _r32(256)
        c.ppt = sbt([128, PP_N], F32, CO, "ppt"); CO += _r32(PP_N * 4)
        c.hpt = sbt([8, 4], F32, CO, "hpt"); CO += _r32(16)
        c.sel = sbt([40, 8, 128], F32, CO, "sel"); CO += _r32(8 * 128 * 4)
        c.m_iu = sbt([128, 128], F32, CO, "m_iu"); CO += _r32(512)
        c.nmu_d = sbt([128, 128], F32, CO, "nmu_d"); CO += _r32(512)
        c.nml_d = sbt([128, 128], F32, CO, "nml_d"); CO += _r32(512)
        c.ml_1 = sbt([128, 128], F32, CO, "ml_1"); CO += _r32(512)
        c.ml_2 = sbt([128, 128], F32, CO, "ml_2"); CO += _r32(512)
        c.cmask = sbt([8, S_LEN], BF16, CO, "cmask"); CO += _r32(S_LEN * 2)
        c.negu2 = sbt([128, 2, 128], F32, CO, "negu2"); CO += _r32(1024)
        c.posl3 = sbt([128, 3, 128], F32, CO, "posl3"); CO += _r32(1536)
        c.epst = sbt([128, 4], F32, CO, "epst"); CO += _r32(16)
        c.cb = Buf("consts")
        c.ARENA = CO
        assert CO <= 131072 + 15872, CO
        c.ARENA = 131072 + 15872

        setup_consts(c)
        S.barrier()
        for L in (0, 1):
            if on("norm%d" % L):
                src = c.x if L == 0 else (c.x1in if (dbg and not on("outp0")) else c.out)
                phase_norm(c, L, src)
                S.barrier()
                if dbg:
                    dump_T(c, c.XT, c.xtd)
                    S.barrier()
            if on("gemm%d" % L):
                phase_gemm(c, L)
                S.barrier()
            if L == 0:
                if on("mixa"):
                    phase_mix_a(c)
                    S.barrier()
                if on("mixb"):
                    phase_mix_b(c)
                    S.barrier()
            else:
                if on("mixc"):
                    phase_mix_c(c)
                    S.barrier()
                if on("mixd"):
                    phase_mix_d(c)
                    S.barrier()
                    for h_ in range(8):
                        S.dma("sp", c.YT[:, 8 + h_, :], c.ydscr[h_], reads=[c.ydb[h_]], writes=[c.ytb[8 + h_]])
                    S.barrier()
            if dbg and (on("mixa") or on("mixb") or on("mixc") or on("mixd")):
                dump_T(c, c.YT, c.ytd)
                S.barrier()
            if on("outp%d" % L):
                if dbg and not (on("mixa") or on("mixb") or on("mixc") or on("mixd")):
                    load_T(c, c.YT, c.ytin)
                    S.barrier()
                src = c.x if L == 0 else (c.x1in if (dbg and not on("outp0")) else c.out)
                phase_outp(c, L, src)
                S.barrier()
        S.emit(block)
    c.nc = nc
    return nc, S


def dump_T(c, T, dst):
    S = c.S
    for s in range(16):
        ev = S.dma("sp", dst[s], T[:, s, :])
    S.barrier()


def load_T(c, T, src):
    S = c.S
    for s in range(16):
        S.dma("sp", T[:, s, :], src[s])


def setup_consts(c):
    S = c.S
    cb = c.cb
    W = [cb]
    S.dma("sp", c.ppt[:], c.pp, writes=W)
    S.dma("sp", c.hpt[:], c.hp, writes=W)
    P = lambda fn: S.op("pool", fn, reads=W, writes=W)
    P(lambda e: e.memset(c.ident[:], 0.0))
    P(lambda e: e.affine_select(out=c.ident[:], in_=c.ident[:], pattern=[[-1, 128]], compare_op=ALU.not_equal, fill=1.0, base=0, channel_multiplier=1))
    P(lambda e: e.memset(c.identf[:], 0.0))
    P(lambda e: e.affine_select(out=c.identf[:], in_=c.identf[:], pattern=[[-1, 128]], compare_op=ALU.not_equal, fill=1.0, base=0, channel_multiplier=1))
    P(lambda e: e.memset(c.ones[:], 1.0))
    P(lambda e: e.memset(c.epst[:, 0:1], EPS))
    P(lambda e: e.memset(c.epst[:, 1:2], 128 * EPS))
    P(lambda e: e.memset(c.epst[:, 2:3], 1.0))
    P(lambda e: e.memset(c.epst[:, 3:4], 0.0))
    P(lambda e: e.memset(c.sel[:], 0.0))
    P(lambda e: e.affine_select(out=c.sel[:], in_=c.sel[:], pattern=[[-1, 8], [0, 128]], compare_op=ALU.not_equal, fill=1.0, base=0, channel_multiplier=1))
    P(lambda e: e.affine_select(out=c.sel[:], in_=c.sel[:], pattern=[[-1, 8], [0, 128]], compare_op=ALU.not_equal, fill=1.0, base=-32, channel_multiplier=1))
    P(lambda e: e.memset(c.cmask[:], 1.0))
    P(lambda e: e.memset(c.cmask[:].rearrange("p (j t) -> p j t", t=128)[:, :, 0:1], 0.0))
    P(lambda e: e.memset(c.m_iu[:], 1.0))
    P(lambda e: e.affine_select(out=c.m_iu[:], in_=c.m_iu[:], pattern=[[1, 128]], compare_op=ALU.is_ge, fill=0.0, base=0, channel_multiplier=-1))
    A = c.ARENA
    su = c.sbt([128, 128], F32, A)
    sl = c.sbt([128, 128], F32, A + 512)
    b32 = c.sbt([128, 128], F32, A + 1024)
    b64 = c.sbt([128, 128], F32, A + 1536)
    e32 = c.sbt([4, 128], F32, A + 2048)
    e64 = c.sbt([2, 128], F32, A + 2560)
    tmp = c.sbt([128, 128], F32, A + 3072)
    P(lambda e: e.memset(su[:], 1.0))
    P(lambda e: e.affine_select(out=su[:], in_=su[:], pattern=[[1, 128]], compare_op=ALU.is_gt, fill=0.0, base=0, channel_multiplier=-1))
    P(lambda e: e.memset(sl[:], 1.0))
    P(lambda e: e.affine_select(out=sl[:], in_=sl[:], pattern=[[-1, 128]], compare_op=ALU.is_gt, fill=0.0, base=0, channel_multiplier=1))
    for (et, bs) in ((e32, 32), (e64, 64)):
        P(lambda e, et=et: e.memset(et[:], 1.0))
        P(lambda e, et=et, bs=bs: e.affine_select(out=et[:], in_=et[:], pattern=[[1, 128]], compare_op=ALU.is_ge, fill=0.0, base=0, channel_multiplier=-bs))
        P(lambda e, et=et, bs=bs: e.affine_select(out=et[:], in_=et[:], pattern=[[-1, 128]], compare_op=ALU.is_ge, fill=0.0, base=bs - 1, channel_multiplier=bs))
    pb = c.psb[0]
    S.op("pe", lambda e: e.matmul(c.ps[0][:, 0:128], lhsT=e32[:], rhs=e32[:], start=True, stop=True), reads=W, writes=[pb])
    S.op("dve", lambda e: e.tensor_copy(out=b32[:], in_=c.ps[0][:, 0:128]), reads=[pb], writes=W)
    S.op("pe", lambda e: e.matmul(c.ps[0][:, 128:256], lhsT=e64[:], rhs=e64[:], start=True, stop=True), reads=W, writes=[pb])
    S.op("dve", lambda e: e.tensor_copy(out=b64[:], in_=c.ps[0][:, 128:256]), reads=[pb], writes=W)
    V = lambda fn: S.op("dve", fn, reads=W, writes=W)
    V(lambda e: e.scalar_tensor_tensor(out=c.nmu_d[:], in0=su[:], scalar=-1.0, in1=b32[:], op0=ALU.mult, op1=ALU.mult))
    V(lambda e: e.scalar_tensor_tensor(out=c.nml_d[:], in0=sl[:], scalar=-1.0, in1=b32[:], op0=ALU.mult, op1=ALU.mult))
    V(lambda e: e.tensor_tensor(out=tmp[:], in0=b64[:], in1=b32[:], op=ALU.subtract))
    V(lambda e: e.tensor_tensor(out=c.ml_1[:], in0=sl[:], in1=tmp[:], op=ALU.mult))
    V(lambda e: e.tensor_scalar(out=tmp[:], in0=b64[:], scalar1=-1.0, scalar2=1.0, op0=ALU.mult, op1=ALU.add))
    V(lambda e: e.tensor_tensor(out=c.ml_2[:], in0=sl[:], in1=tmp[:], op=ALU.mult))
    BIG = 1.0e4
    V(lambda e: e.tensor_scalar(out=c.negu2[:, 0, :], in0=c.m_iu[:], scalar1=BIG, scalar2=-BIG, op0=ALU.mult, op1=ALU.add))
    V(lambda e: e.tensor_scalar(out=c.negu2[:, 1, :], in0=c.nmu_d[:], scalar1=-BIG, scalar2=-BIG, op0=ALU.mult, op1=ALU.add))
    V(lambda e: e.tensor_scalar(out=c.posl3[:, 0, :], in0=c.nml_d[:], scalar1=BIG, scalar2=BIG, op0=ALU.mult, op1=ALU.add))
    V(lambda e: e.tensor_scalar(out=c.posl3[:, 1, :], in0=c.ml_1[:], scalar1=-BIG, scalar2=BIG, op0=ALU.mult, op1=ALU.add))
    V(lambda e: e.tensor_scalar(out=c.posl3[:, 2, :], in0=c.ml_2[:], scalar1=-BIG, scalar2=BIG, op0=ALU.mult, op1=ALU.add))


def phase_norm(c, L, src):
    S = c.S
    A = c.ARENA
    xin = [c.sbt([128, D], F32, A + i * 8192) for i in range(4)]
    xinb = [Buf() for _ in range(4)]
    xs = [c.sbt([128, D], BF16, A + 32768 + i * 4096) for i in range(2)]
    xsb = [Buf() for _ in range(2)]
    junk = c.sbt([128, D], BF16, A + 40960)
    junkb = Buf()
    st = c.sbt([128, 64], F32, A + 45056)
    stb = [Buf() for _ in range(2)]
    goff = PP_EVN if L == 0 else PP_ODN
    for tt in range(NT):
        i = tt % 2
        i3 = tt % 4
        S.dma("sp", xin[i3][:], src[tt * 128:(tt + 1) * 128, :], writes=[xinb[i3]])
        ss = st[:, 2 * i:2 * i + 1]
        rs = st[:, 2 * i + 1:2 * i + 2]
        S.op("act", lambda e, i3=i3, ss=ss: e.activation(out=junk[:], in_=xin[i3][:], func=AF.Square, accum_out=ss), reads=[xinb[i3]], writes=[junkb, stb[i]])
        S.op("act", lambda e, ss=ss, rs=rs: e.activation(out=rs, in_=ss, func=AF.Sqrt, scale=1.0 / D, bias=c.epst[:, 0:1]), reads=[stb[i], c.cb], writes=[stb[i]])
        S.op("dve", lambda e, rs=rs: e.reciprocal(out=rs, in_=rs), reads=[stb[i]], writes=[stb[i]])
        S.op("act", lambda e, i=i, i3=i3, rs=rs: e.activation(out=xs[i][:], in_=xin[i3][:], func=AF.Copy, scale=rs), reads=[xinb[i3], stb[i]], writes=[xsb[i]])
        pbank = [4 + 2 * i, 5 + 2 * i]
        for kc in range(16):
            bk = pbank[kc // 8]
            pv = c.ps[bk][:].bitcast(BF16)
            o = (kc % 8) * 128
            S.op("pe", lambda e, pv=pv, o=o, kc=kc, i=i: e.transpose(out=pv[:, o:o + 128], in_=xs[i][:, kc * 128:(kc + 1) * 128], identity=c.ident[:]),
                 reads=[xsb[i], c.cb], writes=[c.psb[bk]], inc=(kc % 8 == 7))
        for hh in range(2):
            bk = pbank[hh]
            pv = c.ps[bk][:].bitcast(BF16)[:, 0:1024].rearrange("p (k t) -> p k t", t=128)
            g = c.ppt[:, goff + 8 * hh: goff + 8 * hh + 8].unsqueeze(2).to_broadcast([128, 8, 128])
            S.op("dve", lambda e, pv=pv, g=g, hh=hh, tt=tt: e.tensor_tensor(out=c.XT[:, 8 * hh:8 * hh + 8, tt * 128:(tt + 1) * 128], in0=pv, in1=g, op=ALU.mult),
                 reads=[c.psb[bk], c.cb], writes=[c.xtb[tt]])


def phase_gemm(c, L):
    S = c.S
    A = c.ARENA
    A = A + 29 * 1024
    ns = 72 if L == 0 else 48
    ns = min(ns, NS_MAX)
    ntail = 8 if L == 0 else 16
    NW = 3
    wt = [c.sbt([128, 16, 128], BF16, A + i * 4096) for i in range(NW)]
    wtb = [Buf() for _ in range(NW)]
    stg = [c.sbt([128, S_LEN], BF16, A + NW * 4096 + i * 4096) for i in range(2)]
    stgb = [Buf() for _ in range(2)]
    wtl = c.sbt([128, 16, ntail], BF16, A + NW * 4096 + 8192)
    wtlb = Buf()
    stl = c.sbt([ntail, S_LEN], F32, A + NW * 4096 + 8192 + 1024)
    stlb = Buf()
    allx = list(c.xtb)
    dstb = Buf()
    for s in range(min(NW, ns)):
        S.dma("pool", wt[s % NW][:], c.win[L][s], writes=[wtb[s % NW]])
    S.dma("pool", wtl[:], c.wtail[L], writes=[wtlb])
    for s in range(ns):
        wi = s % NW
        for half in range(2):
            banks = [2 * ((2 * s + half) % 2), 2 * ((2 * s + half) % 2) + 1]
            for nb in range(2):
                bk = banks[nb]
                t0 = half * 1024 + nb * 512
                for kc in range(16):
                    S.op("pe", lambda e, bk=bk, wi=wi, kc=kc, t0=t0: e.matmul(c.ps[bk][:, :], lhsT=wt[wi][:, kc, :], rhs=c.XT[:, kc, t0:t0 + 512], start=(kc == 0), stop=(kc == 15)),
                         reads=[wtb[wi]] + allx[t0 // 128: t0 // 128 + 4], writes=[c.psb[bk]], inc=(kc == 15))
                eng = "act" if nb == 0 else "dve"
                if eng == "act":
                    S.op("act", lambda e, bk=bk, t0=t0, s=s: e.activation(out=stg[s % 2][:, t0:t0 + 512], in_=c.ps[bk][:, :], func=AF.Copy), reads=[c.psb[bk]], writes=[stgb[s % 2]])
                else:
                    S.op("dve", lambda e, bk=bk, t0=t0, s=s: e.tensor_copy(out=stg[s % 2][:, t0:t0 + 512], in_=c.ps[bk][:, :]), reads=[c.psb[bk]], writes=[stgb[s % 2]])
        S.dma("sp", c.projT[L][s], stg[s % 2][:], reads=[stgb[s % 2]], writes=[dstb])
        if s + NW < ns:
            S.dma("pool", wt[wi][:], c.win[L][s + NW], writes=[wtb[wi]])
    for nb in range(4):
        bk = nb % 2
        t0 = nb * 512
        for kc in range(16):
            S.op("pe", lambda e, bk=bk, kc=kc, t0=t0: e.matmul(c.ps[bk][0:ntail, :], lhsT=wtl[:, kc, :], rhs=c.XT[:, kc, t0:t0 + 512], start=(kc == 0), stop=(kc == 15)),
                 reads=[wtlb] + allx[t0 // 128: t0 // 128 + 4], writes=[c.psb[bk]], inc=(kc == 15))
        S.op("act", lambda e, bk=bk, t0=t0: e.activation(out=stl[:, t0:t0 + 512], in_=c.ps[bk][0:ntail, :], func=AF.Copy), reads=[c.psb[bk]], writes=[stlb])
    S.dma("sp", c.ptail[L], stl[:], reads=[stlb], writes=[dstb])


def phase_outp(c, L, src):
    S = c.S
    A = c.ARENA
    wo = [c.sbt([128, 16, 512], BF16, A + i * 16384) for i in range(2)]
    wob = [Buf() for _ in range(2)]
    xr = [c.sbt([128, 512], F32, A + 32768 + i * 2048) for i in range(4)]
    xrb = [Buf() for _ in range(4)]
    ally = list(c.ytb)
    if not hasattr(c, "outb"):
        c.outb = [[Buf() for _ in range(4)] for _ in range(NT)]
    S.dma("pool", wo[0][:], c.wout[L][0], writes=[wob[0]])
    it = 0
    for cbk in range(4):
        wi = cbk % 2
        if cbk + 1 < 4:
            S.dma("pool", wo[(cbk + 1) % 2][:], c.wout[L][cbk + 1], writes=[wob[(cbk + 1) % 2]])
        for tt in range(NT):
            xi = it % 4
            bk = it % 4
            it += 1
            rd = [c.outb[tt][cbk]] if src is c.out else []
            S.dma("act", xr[xi][:], src[tt * 128:(tt + 1) * 128, cbk * 512:(cbk + 1) * 512], reads=rd, writes=[xrb[xi]])
            for kc in range(16):
                S.op("pe", lambda e, bk=bk, wi=wi, kc=kc, tt=tt: e.matmul(c.ps[bk][:, :], lhsT=c.YT[:, kc, tt * 128:(tt + 1) * 128], rhs=wo[wi][:, kc, :], start=(kc == 0), stop=(kc == 15)),
                     reads=[wob[wi]] + ally, writes=[c.psb[bk]], inc=(kc == 15))
            S.op("dve", lambda e, bk=bk, xi=xi: e.tensor_tensor(out=xr[xi][:], in0=c.ps[bk][:, :], in1=xr[xi][:], op=ALU.add), reads=[c.psb[bk], xrb[xi]], writes=[xrb[xi]])
            S.dma("sp", c.out[tt * 128:(tt + 1) * 128, cbk * 512:(cbk + 1) * 512], xr[xi][:], reads=[xrb[xi]], writes=[c.outb[tt][cbk]])


def rep_rows(c, dst, dstb, rows, rowsb, h, nrows, bank0=4, p0=0):
    S = c.S
    for nb in range(4):
        bk = bank0 + (nb % 2)
        S.op("pe", lambda e, bk=bk, nb=nb: e.matmul(c.ps[bk][:, :], lhsT=c.sel[p0:p0 + nrows, h, :], rhs=rows[p0:p0 + nrows, nb * 512:(nb + 1) * 512], start=True, stop=True),
             reads=[rowsb, c.cb], writes=[c.psb[bk]])
        S.op("act", lambda e, bk=bk, nb=nb: e.activation(out=dst[:, nb * 512:(nb + 1) * 512], in_=c.ps[bk][:, :], func=AF.Copy), reads=[c.psb[bk]], writes=[dstb])


def cols_from_rows(c, dst, dstb, rows, rowsb, nrows, bank=6):
    S = c.S
    for j in range(NT):
        S.op("pe", lambda e, j=j: e.transpose(out=c.ps[bank][:, j * nrows:(j + 1) * nrows], in_=rows[0:nrows, j * 128:(j + 1) * 128], identity=c.identf[0:nrows, 0:nrows]),
             reads=[rowsb, c.cb], writes=[c.psb[bank]], inc=(j == NT - 1))
    S.op("dve", lambda e: e.tensor_copy(out=dst[:].rearrange("p j r -> p (j r)"), in_=c.ps[bank][:, 0:NT * nrows]), reads=[c.psb[bank]], writes=[dstb])


def sumsq_rstd(c, dst, dstb, srcT, srcb, sq, sqb, scale, bias_ap, banks=(4, 5), c0=0, c1=S_LEN):
    S = c.S
    S.op("pool", lambda e: e.tensor_tensor(out=sq[:, c0:c1], in0=srcT[:, c0:c1], in1=srcT[:, c0:c1], op=ALU.mult), reads=[srcb], writes=[sqb])
    for nb in range(c0 // 512, c1 // 512):
        bk = banks[nb % 2]
        S.op("pe", lambda e, bk=bk, nb=nb: e.matmul(c.ps[bk][:, :], lhsT=c.ones[:], rhs=sq[:, nb * 512:(nb + 1) * 512], start=True, stop=True), reads=[sqb, c.cb], writes=[c.psb[bk]])
        S.op("act", lambda e, bk=bk, nb=nb: e.activation(out=dst[:, nb * 512:(nb + 1) * 512], in_=c.ps[bk][:, :], func=AF.Ln, scale=scale, bias=bias_ap), reads=[c.psb[bk], c.cb], writes=[dstb])
    S.op("act", lambda e: e.activation(out=dst[:, c0:c1], in_=dst[:, c0:c1], func=AF.Exp, scale=-0.5), reads=[dstb], writes=[dstb])


def to_tokmajor(c, dst, dstb, srcT, srcb, bank=6, ncols=128):
    S = c.S
    for g in range(2):
        bk = bank + g
        pv = c.ps[bk][:].bitcast(BF16)
        for jj in range(8):
            j = g * 8 + jj
            S.op("pe", lambda e, pv=pv, jj=jj, j=j: e.transpose(out=pv[:, jj * 128:jj * 128 + ncols], in_=srcT[:, j * 128:(j + 1) * 128], identity=c.ident[:]),
                 reads=[srcb, c.cb], writes=[c.psb[bk]], inc=(jj == 7))
        S.op("act", lambda e, pv=pv, g=g: e.activation(out=dst[:, 8 * g:8 * g + 8, 0:ncols], in_=pv[:, 0:1024].rearrange("p (j d) -> p j d", d=128)[:, :, 0:ncols], func=AF.Copy),
             reads=[c.psb[bk]], writes=[dstb])


def phase_mix_a(c):
    S = c.S
    al = Arena(c)
    qT = [al([128, S_LEN], BF16, 4096) for _ in range(2)]
    kT = [al([128, S_LEN], BF16, 4096) for _ in range(2)]
    vT = [al([128, S_LEN], BF16, 4096) for _ in range(2)]
    zT = [al([128, S_LEN], BF16, 4096) for _ in range(2)]
    lb = [[Buf() for _ in range(4)] for _ in range(2)]
    bt = [al([128, 640], BF16, 1280) for _ in range(2)]
    btb = [Buf() for _ in range(2)]
    sq = al([128, S_LEN], BF16, 4096); sqb = Buf()
    rq = al([128, S_LEN], F32, 8192); rqb = Buf()
    qh = [al([128, S_LEN], BF16, 4096) for _ in range(2)]; qhb = [Buf() for _ in range(2)]
    kh = [al([128, S_LEN], BF16, 4096) for _ in range(2)]; khb = [Buf() for _ in range(2)]
    Vt = [al([128, 16, 128], BF16, 4096) for _ in range(2)]; Vtb = [Buf() for _ in range(2)]
    sz = [al([128, S_LEN], BF16, 4096) for _ in range(2)]; szb = [Buf() for _ in range(2)]
    pT = [al([128, 640], BF16, 1280) for _ in range(2)]; pTb = [Buf() for _ in range(2)]
    rd = [al([128, 128], F32, 512) for _ in range(2)]; rdb = [Buf() for _ in range(2)]
    eps128 = c.epst[:, 1:2]
    epsb = c.epst[:, 0:1]

    def load(h):
        i = h % 2
        for j, (t, base) in enumerate(((qT, 0), (kT, 8), (vT, 16), (zT, 24))):
            S.dma("sp", t[i][:], c.projT[0][base + h], writes=[lb[i][j]])
        S.dma("pool", bt[i][:], c.bt[h], writes=[btb[i]])

    def prologue(h):
        i = h % 2
        th = []

        def norm(srcT, srcb, dst, dstb, scale, bias_ap, gcol):
            th.append(lambda: S.op("pool", lambda e: e.tensor_tensor(out=sq[:], in0=srcT[:], in1=srcT[:], op=ALU.mult), reads=[srcb], writes=[sqb]))
            for nb in range(4):
                bk = 6 + (nb % 2)
                def f(nb=nb, bk=bk):
                    S.op("pe", lambda e: e.matmul(c.ps[bk][:, :], lhsT=c.ones[:], rhs=sq[:, nb * 512:(nb + 1) * 512], start=True, stop=True), reads=[sqb, c.cb], writes=[c.psb[bk]])
                    S.op("act", lambda e: e.activation(out=rq[:, nb * 512:(nb + 1) * 512], in_=c.ps[bk][:, :], func=AF.Ln, scale=scale, bias=bias_ap), reads=[c.psb[bk], c.cb], writes=[rqb])
                th.append(f)
            th.append(lambda: S.op("act", lambda e: e.activation(out=rq[:], in_=rq[:], func=AF.Exp, scale=-0.5), reads=[rqb], writes=[rqb]))
            th.append(lambda: S.op("dve", lambda e: e.scalar_tensor_tensor(out=dst[:], in0=srcT[:], scalar=c.ppt[:, gcol:gcol + 1], in1=rq[:], op0=ALU.mult, op1=ALU.mult), reads=[srcb, rqb, c.cb], writes=[dstb]))
        norm(qT[i], lb[i][0], qh[i], qhb[i], 1.0, eps128, PP_QN)
        norm(kT[i], lb[i][1], kh[i], khb[i], 1.0 / 128, epsb, PP_KN)
        for g in range(2):
            def f(g=g):
                bk = 6 + g
                pv = c.ps[bk][:].bitcast(BF16)
                for jj in range(8):
                    j = g * 8 + jj
                    S.op("pe", lambda e, jj=jj, j=j: e.transpose(out=pv[:, jj * 128:(jj + 1) * 128], in_=vT[i][:, j * 128:(j + 1) * 128], identity=c.ident[:]),
                         reads=[lb[i][2], c.cb], writes=[c.psb[bk]], inc=(jj == 7))
                S.op("act", lambda e: e.activation(out=Vt[i][:, 8 * g:8 * g + 8, :], in_=pv[:, 0:1024].rearrange("p (j d) -> p j d", d=128), func=AF.Copy),
                     reads=[c.psb[bk]], writes=[Vtb[i]])
            th.append(f)
        th.append(lambda: S.op("act", lambda e: e.activation(out=sz[i][:], in_=zT[i][:], func=AF.Silu), reads=[lb[i][3]], writes=[szb[i]]))
        return th

    def stage1(h, n, k):
        i = h % 2
        jb0 = max(0, 4 - n)
        w0 = jb0 * 128
        bks = (0, 1) if k == 0 else (2, 3)
        for jb in range(jb0, 5):
            kb = n - 4 + jb
            bk = bks[0] if jb < 4 else bks[1]
            oo = (jb % 4) * 128
            S.op("pe", lambda e, bk=bk, oo=oo, kb=kb: e.matmul(c.ps[bk][:, oo:oo + 128], lhsT=kh[i][:, kb * 128:(kb + 1) * 128], rhs=qh[i][:, n * 128:(n + 1) * 128], start=True, stop=False),
                 reads=[khb[i], qhb[i]], writes=[c.psb[bk]], inc=False)
            S.op("pe", lambda e, bk=bk, oo=oo, jb=jb: e.matmul(c.ps[bk][:, oo:oo + 128], lhsT=c.ident[:], rhs=bt[i][:, jb * 128:(jb + 1) * 128], start=False, stop=True),
                 reads=[btb[i], c.cb], writes=[c.psb[bk]], inc=(jb == 3 or jb == 4))
        if jb0 < 4:
            S.op("act", lambda e: e.activation(out=pT[k][:, w0:512], in_=c.ps[bks[0]][:, w0:512], func=AF.Exp), reads=[c.psb[bks[0]]], writes=[pTb[k]])
        S.op("act", lambda e: e.activation(out=pT[k][:, 512:640], in_=c.ps[bks[1]][:, 0:128], func=AF.Exp), reads=[c.psb[bks[1]]], writes=[pTb[k]])

    def stage2(h, n, k):
        i = h % 2
        jb0 = max(0, 4 - n)
        bkC = 4 + k
        nj = 5 - jb0
        for idx, jb in enumerate(range(jb0, 5)):
            kb = n - 4 + jb
            S.op("pe", lambda e, kb=kb, jb=jb, idx=idx: e.matmul(c.ps[bkC][:, 0:128], lhsT=Vt[i][:, kb, :], rhs=pT[k][:, jb * 128:(jb + 1) * 128], start=(idx == 0), stop=(idx == nj - 1)),
                 reads=[Vtb[i], pTb[k]], writes=[c.psb[bkC]], inc=False)
        for idx, jb in enumerate(range(jb0, 5)):
            S.op("pe", lambda e, jb=jb, idx=idx: e.matmul(c.ps[bkC][:, 128:256], lhsT=c.ones[:], rhs=pT[k][:, jb * 128:(jb + 1) * 128], start=(idx == 0), stop=(idx == nj - 1)),
                 reads=[c.cb, pTb[k]], writes=[c.psb[bkC]], inc=(idx == nj - 1))
        S.op("dve", lambda e: e.reciprocal(out=rd[k][:], in_=c.ps[bkC][:, 128:256]), reads=[c.psb[bkC]], writes=[rdb[k]])
        S.op("pool", lambda e: e.tensor_tensor(out=rd[k][:], in0=rd[k][:], in1=sz[i][:, n * 128:(n + 1) * 128], op=ALU.mult), reads=[rdb[k], szb[i]], writes=[rdb[k]])
        S.op("dve", lambda e: e.tensor_tensor(out=c.YT[:, h, n * 128:(n + 1) * 128], in0=c.ps[bkC][:, 0:128], in1=rd[k][:], op=ALU.mult), reads=[c.psb[bkC], rdb[k]], writes=[c.ytb[h]])

    load(0)
    for t in prologue(0):
        t()
    its = [(h, n) for h in range(8) for n in range(NT)]
    pend = []
    for idx in range(len(its) + 1):
        if idx < len(its):
            h, n = its[idx]
            if n == 0 and h + 1 < 8:
                load(h + 1)
                pend = prologue(h + 1)
            stage1(h, n, idx % 2)
        if idx >= 1:
            h2, n2 = its[idx - 1]
            stage2(h2, n2, (idx - 1) % 2)
            if n2 >= 2:
                for _ in range(2):
                    if pend:
                        pend.pop(0)()
            if n2 == NT - 1:
                while pend:
                    pend.pop(0)()


def phase_mix_b(c):
    S = c.S
    al = Arena(c)
    gi = al([4, S_LEN], F32, 8192); gib = Buf()
    gf = al([4, S_LEN], F32, 8192); gfb = Buf()
    bb = al([4, S_LEN], F32, 8192); bbb = Buf()
    eb = al([4, S_LEN], F32, 8192); ebb = Buf()
    ek = gi; ekb = gib
    nbf = al([4, 1], F32, 16); nbfb = Buf()
    ekc = al([128, 16, 4], F32, 256); ekcb = Buf()
    S.dma("sp", gi[:], c.ptail[0][0:4, :], writes=[gib])
    S.dma("sp", gf[:], c.ptail[0][4:8, :], writes=[gfb])
    S.op("dve", lambda e: e.tensor_scalar(out=nbf[:], in0=c.hpt[0:4, 1:2], scalar1=-1.0, scalar2=None, op0=ALU.mult), reads=[c.cb], writes=[nbfb])
    S.op("act", lambda e: e.activation(out=gi[:], in_=gi[:], func=AF.Identity, bias=c.hpt[0:4, 0:1]), reads=[gib, c.cb], writes=[gib])
    S.op("act", lambda e: e.activation(out=gf[:], in_=gf[:], func=AF.Exp, scale=-1.0, bias=nbf[:]), reads=[gfb, nbfb], writes=[gfb])
    S.op("act", lambda e: e.activation(out=gf[:], in_=gf[:], func=AF.Ln, bias=c.epst[0:4, 2:3]), reads=[gfb, c.cb], writes=[gfb])
    S.op("dve", lambda e: e.tensor_scalar(out=gf[:], in0=gf[:], scalar1=-1.0, scalar2=None, op0=ALU.mult), reads=[gfb], writes=[gfb])
    S.op("dve", lambda e: e.tensor_tensor_scan(out=bb[:], data0=c.cmask[0:4, :], data1=gf[:], initial=0.0, op0=ALU.mult, op1=ALU.add), reads=[gfb, c.cb], writes=[bbb])
    S.op("act", lambda e: e.activation(out=eb[:], in_=bb[:], func=AF.Exp), reads=[bbb], writes=[ebb])
    S.op("dve", lambda e: e.tensor_tensor(out=ek[:], in0=gi[:], in1=bb[:], op=ALU.subtract), reads=[gib, bbb], writes=[ekb])
    S.op("act", lambda e: e.activation(out=ek[:], in_=ek[:], func=AF.Exp), reads=[ekb], writes=[ekb])
    cols_from_rows(c, ekc, ekcb, ek, ekb, 4)
    EB = al([128, S_LEN], F32, 8192); EBb = Buf()
    EK = al([128, S_LEN], F32, 8192); EKb = Buf()
    ld = [[al([128, S_LEN], BF16, 4096) for _ in range(2)] for _ in range(5)]
    ldb = [[Buf() for _ in range(2)] for _ in range(5)]
    G = [al([128, S_LEN], BF16, 4096) for _ in range(2)]; Gb = [Buf() for _ in range(2)]
    Vx = al([128, 16, 258], BF16, 16 * 258 * 2); Vxb = Buf()
    qs = [al([128, 2, 128], BF16, 512) for _ in range(2)]; qsb = [Buf() for _ in range(2)]
    ksT = [al([128, 2, 128], BF16, 512) for _ in range(2)]; ksTb = [Buf() for _ in range(2)]
    kst = [al([128, 256], BF16, 512) for _ in range(2)]; kstb = [Buf() for _ in range(2)]
    ST = [al([128, 128], BF16, 256) for _ in range(2)]; STb = [Buf() for _ in range(2)]
    Cf = al([128, 2, 258], F32, 2 * 258 * 4); Cfb = Buf()
    Cb = al([128, 2, 256], BF16, 1024); Cbb = Buf()
    Nr = al([128, 2, 128], BF16, 512); Nrb = Buf()
    dd = [al([128, 128], F32, 512) for _ in range(2)]; ddb = [Buf() for _ in range(2)]
    hh_ = [al([128, 2, 128], F32, 1024) for _ in range(2)]; hhb = [Buf() for _ in range(2)]
    base = (32, 40, 48, 56, 64)
    for h in range(4):
        for j in range(5):
            for cc in range(2):
                S.dma("sp", ld[j][cc][:], c.projT[0][base[j] + 2 * h + cc], writes=[ldb[j][cc]])
        rep_rows(c, EB, EBb, eb, ebb, h, 4)
        rep_rows(c, EK, EKb, ek, ekb, h, 4)
        for cc in range(2):
            S.op("act", lambda e, cc=cc: e.activation(out=ld[3][cc][:], in_=ld[3][cc][:], func=AF.Sigmoid), reads=[ldb[3][cc]], writes=[ldb[3][cc]])
            S.op("act", lambda e, cc=cc: e.activation(out=ld[4][cc][:], in_=ld[4][cc][:], func=AF.Silu), reads=[ldb[4][cc]], writes=[ldb[4][cc]])
            S.op("pool", lambda e, cc=cc: e.tensor_tensor(out=G[cc][:], in0=ld[3][cc][:], in1=ld[4][cc][:], op=ALU.mult), reads=[ldb[3][cc], ldb[4][cc]], writes=[Gb[cc]])
        S.op("pool", lambda e: e.memset(Vx[:, :, 256:258], 1.0), writes=[Vxb])
        for cc in range(2):
            for g in range(2):
                bk = 6 + g
                pv = c.ps[bk][:].bitcast(BF16)
                for jj in range(8):
                    j = g * 8 + jj
                    S.op("pe", lambda e, pv=pv, jj=jj, j=j, cc=cc: e.transpose(out=pv[:, jj * 128:(jj + 1) * 128], in_=ld[2][cc][:, j * 128:(j + 1) * 128], identity=c.ident[:]),
                         reads=[ldb[2][cc], c.cb], writes=[c.psb[bk]], inc=(jj == 7))
                S.op("act", lambda e, pv=pv, g=g, cc=cc: e.activation(out=Vx[:, 8 * g:8 * g + 8, cc * 128:(cc + 1) * 128], in_=pv[:, 0:1024].rearrange("p (j d) -> p j d", d=128), func=AF.Copy),
                     reads=[c.psb[bk]], writes=[Vxb])
        S.op("pool", lambda e: e.memset(Cf[:], 0.0), writes=[Cfb])
        S.op("pool", lambda e: e.memset(Cb[:], 0.0), writes=[Cbb])
        S.op("pool", lambda e: e.memset(Nr[:], 0.0), writes=[Nrb])
        for n in range(NT):
            k = n % 2
            tsl = slice(n * 128, (n + 1) * 128)
            for cc in range(2):
                S.op("dve", lambda e, k=k, cc=cc, tsl=tsl: e.scalar_tensor_tensor(out=qs[k][:, cc, :], in0=ld[0][cc][:, tsl], scalar=1.0 / 16.0, in1=EB[:, tsl], op0=ALU.mult, op1=ALU.mult), reads=[ldb[0][cc], EBb], writes=[qsb[k]])
                S.op("pool", lambda e, k=k, cc=cc, tsl=tsl: e.tensor_tensor(out=ksT[k][:, cc, :], in0=ld[1][cc][:, tsl], in1=EK[:, tsl], op=ALU.mult), reads=[ldb[1][cc], EKb], writes=[ksTb[k]])
            bkA = 0 + k
            pv = c.ps[bkA][:].bitcast(BF16)
            for cc in range(2):
                S.op("pe", lambda e, pv=pv, cc=cc, k=k: e.transpose(out=pv[:, cc * 128:(cc + 1) * 128], in_=ksT[k][:, cc, :], identity=c.ident[:]), reads=[ksTb[k], c.cb], writes=[c.psb[bkA]], inc=(cc == 1))
            S.op("act", lambda e, pv=pv, k=k: e.activation(out=kst[k][:], in_=pv[:, 0:256], func=AF.Copy), reads=[c.psb[bkA]], writes=[kstb[k]])
            bkS = 2 + k
            for cc in range(2):
                S.op("pe", lambda e, bkS=bkS, cc=cc, k=k: e.matmul(c.ps[bkS][:, 0:128], lhsT=ksT[k][:, cc, :], rhs=qs[k][:, cc, :], start=(cc == 0), stop=(cc == 1)), reads=[ksTb[k], qsb[k]], writes=[c.psb[bkS]], inc=(cc == 1))
            S.op("dve", lambda e, bkS=bkS, k=k: e.tensor_tensor(out=ST[k][:], in0=c.ps[bkS][:, 0:128], in1=c.m_iu[:], op=ALU.mult), reads=[c.psb[bkS], c.cb], writes=[STb[k]])
            bkN = 4 + k
            for cv in range(2):
                for cc in range(2):
                    S.op("pe", lambda e, bkN=bkN, cv=cv, cc=cc, k=k: e.matmul(c.ps[bkN][:, cv * 128:(cv + 1) * 128], lhsT=Cb[:, cc, cv * 128:(cv + 1) * 128], rhs=qs[k][:, cc, :], start=(cc == 0), stop=False), reads=[Cbb, qsb[k]], writes=[c.psb[bkN]], inc=False)
                S.op("pe", lambda e, bkN=bkN, cv=cv, k=k, n=n: e.matmul(c.ps[bkN][:, cv * 128:(cv + 1) * 128], lhsT=Vx[:, n, cv * 128:(cv + 1) * 128], rhs=ST[k][:], start=False, stop=True), reads=[Vxb, STb[k]], writes=[c.psb[bkN]], inc=False)
            for cc in range(2):
                S.op("pe", lambda e, bkN=bkN, cc=cc, k=k: e.matmul(c.ps[bkN][:, 256:384], lhsT=Nr[:, cc, :], rhs=qs[k][:, cc, :], start=(cc == 0), stop=False), reads=[Nrb, qsb[k]], writes=[c.psb[bkN]], inc=False)
            S.op("pe", lambda e, bkN=bkN, k=k: e.matmul(c.ps[bkN][:, 256:384], lhsT=c.ones[:], rhs=ST[k][:], start=False, stop=True), reads=[c.cb, STb[k]], writes=[c.psb[bkN]])
            S.op("dve", lambda e, bkN=bkN, k=k: e.tensor_scalar(out=dd[k][:], in0=c.ps[bkN][:, 256:384], scalar1=-1.0, scalar2=1.0, op0=ALU.mult, op1=ALU.max), reads=[c.psb[bkN]], writes=[ddb[k]])
            S.op("dve", lambda e, bkN=bkN, k=k: e.tensor_tensor(out=dd[k][:], in0=dd[k][:], in1=c.ps[bkN][:, 256:384], op=ALU.max), reads=[c.psb[bkN], ddb[k]], writes=[ddb[k]])
            S.op("dve", lambda e, k=k: e.reciprocal(out=dd[k][:], in_=dd[k][:]), reads=[ddb[k]], writes=[ddb[k]])
            S.op("dve", lambda e, bkN=bkN, k=k: e.tensor_tensor(out=hh_[k][:], in0=c.ps[bkN][:, 0:256].rearrange("p (c t) -> p c t", t=128), in1=dd[k][:].unsqueeze(1).to_broadcast([128, 2, 128]), op=ALU.mult), reads=[c.psb[bkN], ddb[k]], writes=[hhb[k]])
            for cv in range(2):
                S.op("pool", lambda e, k=k, cv=cv, h=h, tsl=tsl: e.tensor_tensor(out=c.YT[:, 8 + 2 * h + cv, tsl], in0=hh_[k][:, cv, :], in1=G[cv][:, tsl], op=ALU.mult), reads=[hhb[k], Gb[cv]], writes=[c.ytb[8 + 2 * h + cv]])
            if n + 1 < NT:
                ebl = EB[:, n * 128 + 127:n * 128 + 128]
                eblp = EB[:, max(n - 1, 0) * 128 + 127:max(n - 1, 0) * 128 + 128]
                for cc in range(2):
                    bkU = 6 + cc
                    S.op("pe", lambda e, bkU=bkU, cc=cc, k=k, n=n: e.matmul(c.ps[bkU][:, 0:258], lhsT=kst[k][:, cc * 128:(cc + 1) * 128], rhs=Vx[:, n, :], start=True, stop=True), reads=[kstb[k], Vxb], writes=[c.psb[bkU]])
                    S.op("dve", lambda e, cc=cc, bkU=bkU, eblp=eblp: e.scalar_tensor_tensor(out=Cf[:, cc, :], in0=Cf[:, cc, :], scalar=eblp, in1=c.ps[bkU][:, 0:258], op0=ALU.mult, op1=ALU.add), reads=[c.psb[bkU], Cfb, EBb], writes=[Cfb])
                S.op("act", lambda e, ebl=ebl: e.activation(out=Cb[:], in_=Cf[:, :, 0:256], func=AF.Copy, scale=ebl), reads=[Cfb, EBb], writes=[Cbb])
                S.op("act", lambda e, ebl=ebl: e.activation(out=Nr[:], in_=Cf[:, :, 256:257].to_broadcast([128, 2, 128]), func=AF.Copy, scale=ebl), reads=[Cfb, EBb], writes=[Nrb])


def conv4(c, acc, accb, xpad, xpadb, wcol0, bias_ap=None, pad=3, c0=0, c1=S_LEN):
    S = c.S
    w = lambda j: c.ppt[:, wcol0 + j:wcol0 + j + 1]
    p3 = pad - 3
    if bias_ap is not None:
        S.op("act", lambda e: e.activation(out=acc[:, c0:c1], in_=xpad[:, pad + c0:pad + c1], func=AF.Identity, scale=w(3), bias=bias_ap), reads=[xpadb, c.cb], writes=[accb])
    else:
        S.op("act", lambda e: e.activation(out=acc[:, c0:c1], in_=xpad[:, pad + c0:pad + c1], func=AF.Copy, scale=w(3)), reads=[xpadb, c.cb], writes=[accb])
    for j in range(3):
        S.op("dve", lambda e, j=j: e.scalar_tensor_tensor(out=acc[:, c0:c1], in0=xpad[:, p3 + j + c0:p3 + j + c1], scalar=w(j), in1=acc[:, c0:c1], op0=ALU.mult, op1=ALU.add), reads=[xpadb, accb, c.cb], writes=[accb])


def phase_mix_c(c):
    S = c.S
    al = Arena(c)
    xp = [al([128, 3 + S_LEN + 1], BF16, 4104) for _ in range(2)]; xpb = [Buf() for _ in range(2)]
    zt = [al([128, S_LEN], BF16, 4096) for _ in range(2)]; ztb = [Buf() for _ in range(2)]
    gw = [al([128, 256], BF16, 512) for _ in range(2)]; gwb = [Buf() for _ in range(2)]
    acc2 = [al([128, S_LEN], F32, 8192) for _ in range(2)]; accb2 = [Buf() for _ in range(2)]
    xcb2 = [al([128, S_LEN], BF16, 4096) for _ in range(2)]; xcbb2 = [Buf() for _ in range(2)]
    r2 = [al([128, S_LEN], F32, 8192) for _ in range(2)]; rb2 = [Buf() for _ in range(2)]
    i2 = [al([128, S_LEN], F32, 8192) for _ in range(2)]; ib2 = [Buf() for _ in range(2)]
    a2 = [al([128, S_LEN], F32, 8192) for _ in range(2)]; ab2 = [Buf() for _ in range(2)]
    hs2 = [al([128, S_LEN], F32, 8192) for _ in range(2)]; hsb2 = [Buf() for _ in range(2)]
    cc_ = al([128, 8], F32, 32); ccb = Buf()
    S.op("act", lambda e: e.activation(out=cc_[:], in_=c.ppt[:, PP_LAM:PP_LAM + 8], func=AF.Exp, scale=-1.0), reads=[c.cb], writes=[ccb])
    S.op("act", lambda e: e.activation(out=cc_[:], in_=cc_[:], func=AF.Ln, bias=c.epst[:, 2:3]), reads=[ccb, c.cb], writes=[ccb])
    S.op("dve", lambda e: e.tensor_scalar(out=cc_[:], in0=cc_[:], scalar1=-8.0, scalar2=None, op0=ALU.mult), reads=[ccb], writes=[ccb])
    for i in range(2):
        S.op("pool", lambda e, i=i: e.memset(xp[i][:, 0:4], 0.0), writes=[xpb[i]])

    def load(n):
        i = n % 2
        S.dma("sp", xp[i][:, 3:3 + S_LEN], c.projT[1][n], writes=[xpb[i]])
        S.dma("sp", zt[i][:], c.projT[1][8 + n], writes=[ztb[i]])
        S.dma("pool", gw[i][:], c.gatew[n], writes=[gwb[i]])
    load(0)
    for n in range(8):
        i = n % 2
        acc, accb, xcb, xcbb = acc2[i], accb2[i], xcb2[i], xcbb2[i]
        r_, rb, i_, ib, a_, ab, hs, hsb = r2[i], rb2[i], i2[i], ib2[i], a2[i], ab2[i], hs2[i], hsb2[i]
        if n + 1 < 8:
            load(n + 1)
        conv4(c, acc, accb, xp[i], xpb[i], PP_CCW + 4 * n, bias_ap=c.ppt[:, PP_CCB + n:PP_CCB + n + 1])
        S.op("pool", lambda e, xcb=xcb, acc=acc: e.tensor_copy(out=xcb[:], in_=acc[:]), reads=[accb], writes=[xcbb])
        for part, dst, dstb, bcol in ((0, r_, rb, PP_GBR), (1, i_, ib, PP_GBI)):
            for nb in range(4):
                bk = (part * 4 + nb) % 8
                S.op("pe", lambda e, bk=bk, part=part, nb=nb, i=i, xcb=xcb: e.matmul(c.ps[bk][:, :], lhsT=gw[i][:, part * 128:(part + 1) * 128], rhs=xcb[:, nb * 512:(nb + 1) * 512], start=True, stop=True), reads=[gwb[i], xcbb], writes=[c.psb[bk]])
                S.op("act", lambda e, bk=bk, nb=nb, dst=dst, bcol=bcol, n=n: e.activation(out=dst[:, nb * 512:(nb + 1) * 512], in_=c.ps[bk][:, :], func=AF.Sigmoid, bias=c.ppt[:, bcol + n:bcol + n + 1]), reads=[c.psb[bk], c.cb], writes=[dstb])
        S.op("act", lambda e, n=n, a_=a_, r_=r_: e.activation(out=a_[:], in_=r_[:], func=AF.Exp, scale=cc_[:, n:n + 1]), reads=[rb, ccb], writes=[ab])
        S.op("pool", lambda e, a_=a_, r_=r_: e.tensor_tensor(out=r_[:], in0=a_[:], in1=a_[:], op=ALU.mult), reads=[ab, rb], writes=[rb])
        S.op("act", lambda e, r_=r_: e.activation(out=r_[:], in_=r_[:], func=AF.Sqrt, scale=-1.0, bias=c.epst[:, 2:3]), reads=[rb, c.cb], writes=[rb])
        S.op("dve", lambda e, i_=i_, acc=acc: e.tensor_tensor(out=i_[:], in0=i_[:], in1=acc[:], op=ALU.mult), reads=[ib, accb], writes=[ib])
        S.op("dve", lambda e, i_=i_, r_=r_: e.tensor_tensor(out=i_[:], in0=i_[:], in1=r_[:], op=ALU.mult), reads=[ib, rb], writes=[ib])
        S.op("dve", lambda e, hs=hs, a_=a_, i_=i_: e.tensor_tensor_scan(out=hs[:], data0=a_[:], data1=i_[:], initial=0.0, op0=ALU.mult, op1=ALU.add), reads=[ab, ib], writes=[hsb])
        S.op("act", lambda e, i=i: e.activation(out=zt[i][:], in_=zt[i][:], func=AF.Silu), reads=[ztb[i]], writes=[ztb[i]])
        S.op("pool", lambda e, i=i, n=n, hs=hs: e.tensor_tensor(out=c.YT[:, n, :], in0=hs[:], in1=zt[i][:], op=ALU.mult), reads=[hsb, ztb[i]], writes=[c.ytb[n]])


def phase_mix_d(c):
    S = c.S
    al = Arena(c, segs=[(0, 65536), (65536 + 32768, 131072), (c.ARENA, SBUF_BYTES)])
    G = 4
    NGR = NT // G
    odT, odT_off = al.at([128, S_LEN], F32, 8192); odTb = Buf()
    RD = al([40, S_LEN], F32, 8192); RDb = Buf(); RBb = Buf()
    nea = al([8, 1], F32, 32); neab = Buf()
    cols = al([128, 16, 16], F32, 1024); colsb = Buf()
    c3 = al([128, 16, 8, 2], F32, 1024); c3b = Buf()
    dlr2 = [al([128, 16], F32, 64) for _ in range(2)]; dlrb2 = [Buf(), Buf()]
    tmpc = al([128, 16, 8], F32, 512); tmpcb = Buf()
    S.dma("sp", odT[0:8, :], c.ptail[1][0:8, :], writes=[odTb])
    S.dma("sp", RD[32:40, :], c.ptail[1][8:16, :], writes=[RBb])
    S.op("act", lambda e: e.activation(out=nea[:], in_=c.hpt[0:8, 2:3], func=AF.Exp), reads=[c.cb], writes=[neab])
    S.op("dve", lambda e: e.tensor_scalar(out=nea[:], in0=nea[:], scalar1=-1.0, scalar2=None, op0=ALU.mult), reads=[neab], writes=[neab])
    S.op("act", lambda e: e.activation(out=odT[0:8, :], in_=odT[0:8, :], func=AF.Exp, bias=c.hpt[0:8, 3:4]), reads=[odTb, c.cb], writes=[odTb])
    S.op("act", lambda e: e.activation(out=odT[0:8, :], in_=odT[0:8, :], func=AF.Ln, bias=c.epst[0:8, 2:3]), reads=[odTb, c.cb], writes=[odTb])
    S.op("dve", lambda e: e.tensor_scalar(out=odT[0:8, :], in0=odT[0:8, :], scalar1=nea[:], scalar2=None, op0=ALU.mult), reads=[odTb, neab], writes=[odTb])
    S.op("dve", lambda e: e.tensor_tensor_scan(out=RD[0:8, :], data0=c.cmask[0:8, :], data1=odT[0:8, :], initial=0.0, op0=ALU.mult, op1=ALU.add), reads=[odTb, c.cb], writes=[RDb])
    S.op("act", lambda e: e.activation(out=RD[32:40, :], in_=RD[32:40, :], func=AF.Sigmoid), reads=[RBb], writes=[RBb])
    for (p0, rb_, off) in ((32, RBb, 0), (0, RDb, 8)):
        for j in range(NT):
            S.op("pe", lambda e, j=j, p0=p0: e.transpose(out=c.ps[6][:, j * 8:(j + 1) * 8], in_=RD[p0:p0 + 8, j * 128:(j + 1) * 128], identity=c.identf[p0:p0 + 8, p0:p0 + 8]), reads=[rb_, c.cb], writes=[c.psb[6]], inc=(j == NT - 1))
        S.op("dve", lambda e, off=off: e.tensor_copy(out=cols[:, :, off:off + 8], in_=c.ps[6][:, 0:128].rearrange("p (j r) -> p j r", r=8)), reads=[c.psb[6]], writes=[colsb])
    S.op("act", lambda e: e.activation(out=tmpc[:], in_=cols[:, :, 8:16], func=AF.Exp), reads=[colsb], writes=[tmpcb])
    S.op("dve", lambda e: e.tensor_tensor(out=c3[:, :, :, 0], in0=tmpc[:], in1=cols[:, :, 0:8], op=ALU.mult), reads=[tmpcb, colsb], writes=[c3b])
    DR = al([128, S_LEN], F32, 8192); DRb = Buf()
    BR, BR_off = al.at([128, S_LEN], F32, 8192); BRb = Buf()
    ARG = c.sbt([128, G, 3, 128], F32, BR_off)
    GUm = al([128, 16, 128], BF16, 4096); GUmb = [Buf() for _ in range(NT // 4)]
    qT = al([128, S_LEN], BF16, 4096); qTb = Buf()
    kT = al([128, S_LEN], BF16, 4096); kTb = Buf()
    kTB, kTB_off = al.at([128, S_LEN], BF16, 4096); kTBb = Buf()
    zt = al([128, S_LEN], BF16, 4096); ztb = Buf()
    Kt = al([128, 16, 128], BF16, 4096); Ktb = Buf()
    Vt = al([128, 16, 128], BF16, 4096); Vtb = Buf()
    Xbb = [Buf() for _ in range(NGR)]
    xp0, xp0_off = al.at([128, 16 + S_LEN], BF16, 4128)
    xp1, xp1_off = al.at([128, 16 + S_LEN], BF16, 4128)
    Xb = al([128, 16, 128], BF16, 4096)
    xp = [xp0, xp1, xp0]; xpb = [Buf(), Buf()]; xpb.append(xpb[0])
    edq = c.sbt([128, S_LEN], BF16, xp0_off + 32)
    acc, acc_off = al.at([128, S_LEN], F32, 8192); accb = Buf()
    vb_all = al([128, 16, 128], BF16, 4096); vbab = Buf()
    kbe_all = al([128, 16, 128], BF16, 4096); kbeb = Buf()
    rn, rn_off = al.at([128, S_LEN], F32, 8192); rnb = Buf()
    kd_all = al([128, 16, 128], BF16, 4096); kdab = Buf()
    qd_all = al([128, S_LEN], BF16, 4096); qdab = Buf()
    sq, sq_off = al.at([128, S_LEN], BF16, 4096); sqb = Buf()
    nw_all = al([128, 16, 128], BF16, 4096)
    vT, vT_off = al.at([128, S_LEN], BF16, 4096); vTb = Buf()
    qk_all = al([128, 16, 128], BF16, 4096); qkb = [Buf() for _ in range(NT // 4)]
    sqf = [al([128, 512], BF16, 1024)] * 2; sqfb = [Buf()] * 2
    rnf = [al([128, 512], F32, 2048)] * 2; rnfb = [Buf()] * 2
    nwb = [Buf() for _ in range(NT // 4)]
    al.segs[0][0] = al.segs[0][1]
    ED = [al([128, G, 128], BF16, 1024) for _ in range(2)]; EDb = [Buf() for _ in range(2)]
    EA = [al([128, G, 3, 128], BF16, 3072) for _ in range(2)]; EAb = [Buf() for _ in range(2)]
    Nf = [al([128, G, 128], F32, 2048) for _ in range(2)]; Nfb = [Buf() for _ in range(2)]
    MM = [[al([128, 2, G, 128], BF16, 2048) for _ in range(2)] for _ in range(2)]; MMb = [[[Buf(), Buf()] for _ in range(2)] for _ in range(2)]
    R12 = [al([128, G, 2, 128], BF16, 2048) for _ in range(2)]; R12b = [Buf() for _ in range(2)]
    Pb2 = [[al([128, G, 128], BF16, 1024) for _ in range(2)] for _ in range(2)]; Pbb2 = [[Buf() for _ in range(2)] for _ in range(2)]
    PT = [al([128, G, 128], BF16, 1024) for _ in range(2)]; PTb = [Buf() for _ in range(2)]
    Yb = [al([128, G, 128], BF16, 1024) for _ in range(2)]; Ybb = [Buf() for _ in range(2)]
    vn_ = [al([128, 128], BF16, 256) for _ in range(2)]; vnb = [Buf() for _ in range(2)]
    Sf = al([128, 128], F32, 512); Sfb = Buf()
    Sb = [al([128, 128], BF16, 256) for _ in range(2)]; Sbb = [Buf() for _ in range(2)]
    dummy = al([128, 8], F32, 32); dummyb = Buf()
    acch = [Buf(), Buf()]; rnh = [Buf(), Buf()]; sqh = [Buf(), Buf()]
    for i in range(2):
        S.op("pool", lambda e, i=i: e.memset(xp[i][:, 0:16], 0.0), writes=[xpb[i]])
    eps_ap = c.epst[:, 0:1]
    DR3 = DR[:].rearrange("p (j t) -> p j t", t=128)
    identG = c.identf[:].unsqueeze(1).to_broadcast([128, G, 128])
    for h in range(8):
        S.dma("sp", xp[0][:, 16:16 + S_LEN], c.projT[1][16 + h], writes=[xpb[0]])
        S.dma("sp", xp[1][:, 16:16 + S_LEN], c.projT[1][24 + h], writes=[xpb[1]])
        S.dma("sp", zt[:], c.projT[1][40 + h], writes=[ztb])
        dlr = dlr2[h % 2]; dlrb = dlrb2[h % 2]
        rep_rows(c, DR, DRb, RD, RDb, h, 8, p0=0)
        rep_rows(c, BR, BRb, RD, RBb, h, 8, p0=32)
        S.op("act", lambda e, dlr=dlr: e.activation(out=dlr[:], in_=DR3[:, :, 127], func=AF.Exp), reads=[DRb], writes=[dlrb])
        S.op("dve", lambda e, h=h: e.tensor_tensor(out=tmpc[:, :, h], in0=DR3[:, :, 127], in1=cols[:, :, 8 + h], op=ALU.subtract), reads=[DRb, colsb], writes=[tmpcb])
        S.op("act", lambda e, h=h: e.activation(out=c3[:, :, h, 1], in_=tmpc[:, :, h], func=AF.Exp), reads=[tmpcb], writes=[c3b])
        fence_w = [accb, rnb, sqb] + acch + rnh + sqh
        S.op("pool", lambda e: e.memset(dummy[:], 0.0), writes=fence_w + [dummyb])
        for which, dstT, dstTb in ((0, qT, qTb), (1, kT, kTb), (2, vT, vTb)):
            if which == 2:
                S.dma("sp", xp[2][:, 16:16 + S_LEN], c.projT[1][32 + h], writes=[xpb[2]])
            for hf in range(2):
                c0, c1 = hf * 1024, (hf + 1) * 1024
                ab_, rb_, sb_ = acch[hf], rnh[hf], sqh[hf]
                conv4(c, acc, ab_, xp[which], xpb[which], PP_CDW + 4 * (8 * which + h), pad=16, c0=c0, c1=c1)
                if which == 2:
                    S.op("act", lambda e, c0=c0, c1=c1: e.activation(out=vT[:, c0:c1], in_=acc[:, c0:c1], func=AF.Silu), reads=[ab_], writes=[vTb])
                else:
                    S.op("act", lambda e, c0=c0, c1=c1: e.activation(out=acc[:, c0:c1], in_=acc[:, c0:c1], func=AF.Silu), reads=[ab_], writes=[ab_])
                    sumsq_rstd(c, rn, rb_, acc, ab_, sq, sb_, 1.0, eps_ap, c0=c0, c1=c1)
                    if which == 0:
                        S.op("dve", lambda e, dstT=dstT, c0=c0, c1=c1: e.scalar_tensor_tensor(out=dstT[:, c0:c1], in0=acc[:, c0:c1], scalar=float(128 ** -0.5), in1=rn[:, c0:c1], op0=ALU.mult, op1=ALU.mult), reads=[ab_, rb_], writes=[dstTb])
                    else:
                        S.op("dve", lambda e, dstT=dstT, c0=c0, c1=c1: e.tensor_tensor(out=dstT[:, c0:c1], in0=acc[:, c0:c1], in1=rn[:, c0:c1], op=ALU.mult), reads=[ab_, rb_], writes=[dstTb])
        S.op("pool", lambda e: e.memset(dummy[:], 0.0), writes=fence_w + [dummyb])
        S.op("pool", lambda e: e.tensor_tensor(out=kTB[:], in0=kT[:], in1=BR[:], op=ALU.mult), reads=[kTb, BRb], writes=[kTBb])
        S.op("act", lambda e: e.activation(out=edq[:], in_=DR[:], func=AF.Exp), reads=[DRb, xpb[0]], writes=[xpb[0]])
        S.op("pool", lambda e: e.tensor_tensor(out=qd_all[:], in0=qT[:], in1=edq[:], op=ALU.mult), reads=[qTb, xpb[0]], writes=[qdab])
        to_tokmajor(c, Kt, Ktb, kT, kTb)
        to_tokmajor(c, Vt, Vtb, vT, vTb)
        bc = lambda ap: ap.to_broadcast([128, 16, 128])
        S.op("dve", lambda e, h=h: e.tensor_tensor(out=vb_all[:], in0=Vt[:], in1=bc(cols[:, :, h:h + 1]), op=ALU.mult), reads=[Vtb, colsb], writes=[vbab])
        S.op("pool", lambda e, h=h: e.tensor_tensor(out=kbe_all[:], in0=Kt[:], in1=bc(c3[:, :, h, 0:1]), op=ALU.mult), reads=[Ktb, c3b], writes=[kbeb])
        S.op("dve", lambda e, h=h: e.tensor_tensor(out=kd_all[:], in0=Kt[:], in1=bc(c3[:, :, h, 1:2]), op=ALU.mult), reads=[Ktb, c3b], writes=[kdab])
        for gi in range(NGR):
            k = gi % 2
            b0, b1, b2, b3 = 4 * k, 4 * k + 1, 4 * k + 2, 4 * k + 3
            blks = range(G * gi, G * gi + G)
            pv3 = c.ps[b3][:].bitcast(BF16)
            for bi, n in enumerate(blks):
                tsl = slice(n * 128, (n + 1) * 128)
                dcol = cols[:, n, 8 + h:9 + h]
                S.op("dve", lambda e, b0=b0, b1=b1, b2=b2, b3=b3, pv3=pv3, bi=bi, tsl=tsl, dcol=dcol: e.scalar_tensor_tensor(out=ARG[:, bi, 0:2, :], in0=DR[:, tsl].unsqueeze(1).to_broadcast([128, 2, 128]), scalar=dcol, in1=c.negu2[:], op0=ALU.subtract, op1=ALU.min),
                     reads=[DRb, colsb, BRb, c.cb], writes=[BRb])
            S.op("act", lambda e, b0=b0, b1=b1, b2=b2, b3=b3, pv3=pv3, gi=gi: e.activation(out=GUm[:, G * gi:G * gi + G, :], in_=ARG[:, :, 0, :], func=AF.Exp), reads=[BRb], writes=[GUmb[gi]])
            S.op("act", lambda e, b0=b0, b1=b1, b2=b2, b3=b3, pv3=pv3, k=k: e.activation(out=ED[k][:], in_=ARG[:, :, 1, :], func=AF.Exp), reads=[BRb], writes=[EDb[k]])
            for bi, n in enumerate(blks):
                tsl = slice(n * 128, (n + 1) * 128)
                dcol = cols[:, n, 8 + h:9 + h]
                S.op("dve", lambda e, b0=b0, b1=b1, b2=b2, b3=b3, pv3=pv3, bi=bi, tsl=tsl, dcol=dcol: e.scalar_tensor_tensor(out=ARG[:, bi, :, :], in0=DR[:, tsl].unsqueeze(1).to_broadcast([128, 3, 128]), scalar=dcol, in1=c.posl3[:], op0=ALU.subtract, op1=ALU.max),
                     reads=[DRb, colsb, BRb, c.cb], writes=[BRb])
            S.op("act", lambda e, b0=b0, b1=b1, b2=b2, b3=b3, pv3=pv3, k=k: e.activation(out=EA[k][:], in_=ARG[:], func=AF.Exp, scale=-1.0), reads=[BRb], writes=[EAb[k]])
            for bi, n in enumerate(blks):
                tsl = slice(n * 128, (n + 1) * 128)
                S.op("pe", lambda e, b0=b0, b1=b1, b2=b2, b3=b3, pv3=pv3, bi=bi, tsl=tsl: e.matmul(c.ps[b0][:, bi * 128:(bi + 1) * 128], lhsT=kT[:, tsl], rhs=kTB[:, tsl], start=True, stop=True), reads=[kTb, kTBb], writes=[c.psb[b0]], inc=(bi == G - 1))
            for bi, n in enumerate(blks):
                tsl = slice(n * 128, (n + 1) * 128)
                S.op("pe", lambda e, b0=b0, b1=b1, b2=b2, b3=b3, pv3=pv3, bi=bi, tsl=tsl: e.matmul(c.ps[b1][:, bi * 128:(bi + 1) * 128], lhsT=kTB[:, tsl], rhs=kT[:, tsl], start=True, stop=True), reads=[kTb, kTBb], writes=[c.psb[b1]], inc=(bi == G - 1))
            pv0 = c.ps[b0][:, :].rearrange("p (g t) -> p g t", t=128)
            pv1 = c.ps[b1][:, :].rearrange("p (g t) -> p g t", t=128)
            pv2 = c.ps[b2][:, :].rearrange("p (g t) -> p g t", t=128)
            S.op("dve", lambda e, b0=b0, b1=b1, b2=b2, b3=b3, pv3=pv3, k=k, pv0=pv0: e.scalar_tensor_tensor(out=Nf[k][:], in0=pv0, scalar=-1.0, in1=ED[k][:], op0=ALU.mult, op1=ALU.mult), reads=[c.psb[b0], EDb[k]], writes=[Nfb[k]])
            S.op("act", lambda e, b0=b0, b1=b1, b2=b2, b3=b3, pv3=pv3, k=k: e.activation(out=MM[k][0][:, 0], in_=Nf[k][:], func=AF.Copy), reads=[Nfb[k]], writes=[MMb[k][0][0]])
            S.op("dve", lambda e, b0=b0, b1=b1, b2=b2, b3=b3, pv3=pv3, k=k, pv1=pv1: e.scalar_tensor_tensor(out=MM[k][0][:, 1], in0=pv1, scalar=-1.0, in1=EA[k][:, :, 0, :], op0=ALU.mult, op1=ALU.mult), reads=[c.psb[b1], EAb[k]], writes=[MMb[k][0][1]])
            S.op("dve", lambda e, b0=b0, b1=b1, b2=b2, b3=b3, pv3=pv3, k=k, pv1=pv1: e.tensor_tensor(out=R12[k][:], in0=pv1.unsqueeze(2).to_broadcast([128, G, 2, 128]), in1=EA[k][:, :, 1:3, :], op=ALU.mult), reads=[c.psb[b1], EAb[k]], writes=[R12b[k]])
            pc = 0
            S.op("pool", lambda e, k=k: e.tensor_tensor(out=Pb2[k][0][:], in0=Nf[k][:], in1=identG, op=ALU.add), reads=[Nfb[k], c.cb], writes=[Pbb2[k][0]])
            cur = 0
            for lvl in range(4):
                nxt = 1 - cur
                last = (lvl == 3)
                for bi in range(G):
                    S.op("pe", lambda e, b1=b1, bi=bi, k=k, cur=cur: e.matmul(c.ps[b1][:, bi * 128:(bi + 1) * 128], lhsT=MM[k][cur][:, 0, bi, :], rhs=MM[k][cur][:, 1, bi, :], start=True, stop=True),
                         reads=MMb[k][cur], writes=[c.psb[b1]], inc=(bi == G - 1))
                if not last:
                    for bi in range(G):
                        S.op("pe", lambda e, b0=b0, bi=bi, k=k, cur=cur: e.matmul(c.ps[b0][:, bi * 128:(bi + 1) * 128], lhsT=MM[k][cur][:, 1, bi, :], rhs=MM[k][cur][:, 0, bi, :], start=True, stop=True),
                             reads=MMb[k][cur], writes=[c.psb[b0]], inc=(bi == G - 1))
                S.op("act", lambda e, k=k, nxt=nxt, pv1=pv1: e.activation(out=MM[k][nxt][:, 1], in_=pv1, func=AF.Copy), reads=[c.psb[b1]], writes=[MMb[k][nxt][1]])
                if not last:
                    S.op("act", lambda e, k=k, nxt=nxt, pv0=pv0: e.activation(out=MM[k][nxt][:, 0], in_=pv0, func=AF.Copy), reads=[c.psb[b0]], writes=[MMb[k][nxt][0]])
                for bi in range(G):
                    S.op("pe", lambda e, b2=b2, bi=bi, k=k, nxt=nxt, pc=pc: e.matmul(c.ps[b2][:, bi * 128:(bi + 1) * 128], lhsT=MM[k][nxt][:, 1, bi, :], rhs=Pb2[k][pc][:, bi, :], start=True, stop=False),
                         reads=[MMb[k][nxt][1], Pbb2[k][pc]], writes=[c.psb[b2]], inc=False)
                    S.op("pe", lambda e, b2=b2, bi=bi, k=k, pc=pc: e.matmul(c.ps[b2][:, bi * 128:(bi + 1) * 128], lhsT=c.ident[:], rhs=Pb2[k][pc][:, bi, :], start=False, stop=True),
                         reads=[c.cb, Pbb2[k][pc]], writes=[c.psb[b2]], inc=(bi == G - 1))
                S.op("act", lambda e, k=k, pv2=pv2, pc=pc: e.activation(out=Pb2[k][1 - pc][:], in_=pv2, func=AF.Copy), reads=[c.psb[b2]], writes=[Pbb2[k][1 - pc]])
                pc = 1 - pc
                cur = nxt
            for r in range(2):
                for bi in range(G):
                    S.op("pe", lambda e, pv3=pv3, bi=bi, k=k, pc=pc: e.transpose(out=pv3[:, bi * 128:(bi + 1) * 128], in_=Pb2[k][pc][:, bi, :], identity=c.ident[:]), reads=[Pbb2[k][pc], c.cb], writes=[c.psb[b3]], inc=(bi == G - 1))
                S.op("act", lambda e, pv3=pv3, k=k: e.activation(out=PT[k][:], in_=pv3[:, 0:G * 128].rearrange("p (g t) -> p g t", t=128), func=AF.Copy), reads=[c.psb[b3]], writes=[PTb[k]])
                for bi in range(G):
                    S.op("pe", lambda e, b1=b1, bi=bi, k=k, r=r, pc=pc: e.matmul(c.ps[b1][:, bi * 128:(bi + 1) * 128], lhsT=R12[k][:, bi, r, :], rhs=Pb2[k][pc][:, bi, :], start=True, stop=True), reads=[R12b[k], Pbb2[k][pc]], writes=[c.psb[b1]], inc=(bi == G - 1))
                S.op("act", lambda e, k=k, pv1=pv1: e.activation(out=Yb[k][:], in_=pv1, func=AF.Copy, scale=-1.0), reads=[c.psb[b1]], writes=[Ybb[k]])
                for bi in range(G):
                    S.op("pe", lambda e, b0=b0, bi=bi, k=k, pc=pc: e.matmul(c.ps[b0][:, bi * 128:(bi + 1) * 128], lhsT=c.ident[:], rhs=Pb2[k][pc][:, bi, :], start=True, stop=False), reads=[c.cb, Pbb2[k][pc]], writes=[c.psb[b0]], inc=False)
                    S.op("pe", lambda e, b0=b0, bi=bi, k=k: e.matmul(c.ps[b0][:, bi * 128:(bi + 1) * 128], lhsT=PT[k][:, bi, :], rhs=Yb[k][:, bi, :], start=False, stop=True), reads=[PTb[k], Ybb[k]], writes=[c.psb[b0]], inc=(bi == G - 1))
                if r == 1:
                    S.op("act", lambda e, k=k, gi=gi, pv0=pv0: e.activation(out=Xb[:, G * gi:G * gi + G, :], in_=pv0, func=AF.Copy), reads=[c.psb[b0]], writes=[Xbb[gi]])
                else:
                    S.op("act", lambda e, k=k, pv0=pv0, pc=pc: e.activation(out=Pb2[k][1 - pc][:], in_=pv0, func=AF.Copy), reads=[c.psb[b0]], writes=[Pbb2[k][1 - pc]])
                    pc = 1 - pc
            for bi, n in enumerate(blks):
                tsl = slice(n * 128, (n + 1) * 128)
                S.op("pe", lambda e, b0=b0, b1=b1, b2=b2, b3=b3, pv3=pv3, bi=bi, tsl=tsl: e.matmul(c.ps[b1][:, bi * 128:(bi + 1) * 128], lhsT=kT[:, tsl], rhs=qT[:, tsl], start=True, stop=True), reads=[kTb, qTb], writes=[c.psb[b1]], inc=(bi == G - 1))
            S.op("dve", lambda e, b0=b0, b1=b1, b2=b2, b3=b3, pv3=pv3, gi=gi, pv1=pv1: e.tensor_tensor(out=qk_all[:, G * gi:G * gi + G, :], in0=pv1, in1=GUm[:, G * gi:G * gi + G, :], op=ALU.mult), reads=[c.psb[b1], GUmb[gi]], writes=[qkb[gi]])
            for bi, n in enumerate(blks):
                S.op("pe", lambda e, b0=b0, b1=b1, b2=b2, b3=b3, pv3=pv3, bi=bi, n=n: e.matmul(c.ps[b2][:, bi * 128:(bi + 1) * 128], lhsT=kbe_all[:, n, :], rhs=Xb[:, n, :], start=True, stop=True), reads=[kbeb, Xbb[gi]], writes=[c.psb[b2]], inc=(bi == G - 1))
            S.op("act", lambda e, b0=b0, b1=b1, b2=b2, b3=b3, pv3=pv3, gi=gi, pv2=pv2: e.activation(out=nw_all[:, G * gi:G * gi + G, :], in_=pv2, func=AF.Copy, scale=-1.0), reads=[c.psb[b2]], writes=[nwb[gi]])
        S.op("pool", lambda e: e.memset(Sf[:], 0.0), writes=[Sfb])
        S.op("pool", lambda e: e.memset(Sb[0][:], 0.0), writes=[Sbb[0]])
        for n in range(NT):
            k = n % 2
            gi = n // G
            tsl = slice(n * 128, (n + 1) * 128)
            bkB, bkC, bkD = 0 + k, 2 + k, 4 + k
            sc_, sn_ = Sb[k], Sb[1 - k]
            scb_, snb_ = Sbb[k], Sbb[1 - k]
            S.op("pe", lambda e, bkB=bkB, n=n: e.matmul(c.ps[bkB][:, 0:128], lhsT=Xb[:, n, :], rhs=vb_all[:, n, :], start=True, stop=False), reads=[Xbb[gi], vbab], writes=[c.psb[bkB]], inc=False)
            S.op("pe", lambda e, bkB=bkB, n=n, sc_=sc_: e.matmul(c.ps[bkB][:, 0:128], lhsT=nw_all[:, n, :], rhs=sc_[:], start=False, stop=True), reads=[nwb[gi], scb_], writes=[c.psb[bkB]])
            S.op("act", lambda e, bkB=bkB, k=k: e.activation(out=vn_[k][:], in_=c.ps[bkB][:, 0:128], func=AF.Copy), reads=[c.psb[bkB]], writes=[vnb[k]])
            if n + 1 < NT:
                S.op("pe", lambda e, bkD=bkD, k=k, n=n: e.matmul(c.ps[bkD][:, 0:128], lhsT=kd_all[:, n, :], rhs=vn_[k][:], start=True, stop=True), reads=[kdab, vnb[k]], writes=[c.psb[bkD]])
                S.op("dve", lambda e, bkD=bkD, n=n, dlr=dlr: e.scalar_tensor_tensor(out=Sf[:], in0=Sf[:], scalar=dlr[:, n:n + 1], in1=c.ps[bkD][:, 0:128], op0=ALU.mult, op1=ALU.add), reads=[c.psb[bkD], Sfb, dlrb], writes=[Sfb])
                S.op("act", lambda e, sn_=sn_: e.activation(out=sn_[:], in_=Sf[:], func=AF.Copy), reads=[Sfb], writes=[snb_])
            S.op("pe", lambda e, bkC=bkC, tsl=tsl, sc_=sc_: e.matmul(c.ps[bkC][:, 0:128], lhsT=sc_[:], rhs=qd_all[:, tsl], start=True, stop=False), reads=[scb_, qdab], writes=[c.psb[bkC]], inc=False)
            S.op("pe", lambda e, bkC=bkC, k=k, n=n: e.matmul(c.ps[bkC][:, 0:128], lhsT=vn_[k][:], rhs=qk_all[:, n, :], start=False, stop=True), reads=[vnb[k], qkb[gi]], writes=[c.psb[bkC]])
            S.op("dve", lambda e, bkC=bkC, tsl=tsl: e.tensor_copy(out=odT[:, tsl], in_=c.ps[bkC][:, 0:128]), reads=[c.psb[bkC]], writes=[odTb])
        S.op("act", lambda e: e.activation(out=zt[:], in_=zt[:], func=AF.Silu), reads=[ztb], writes=[ztb])
        for nb in range(4):
            kk = nb % 2
            cs = slice(nb * 512, (nb + 1) * 512)
            bk = 6 + kk
            S.op("pool", lambda e, kk=kk, cs=cs: e.tensor_tensor(out=sqf[kk][:], in0=odT[:, cs], in1=odT[:, cs], op=ALU.mult), reads=[odTb], writes=[sqfb[kk]])
            S.op("pe", lambda e, kk=kk, bk=bk: e.matmul(c.ps[bk][:, :], lhsT=c.ones[:], rhs=sqf[kk][:], start=True, stop=True), reads=[sqfb[kk], c.cb], writes=[c.psb[bk]])
            S.op("act", lambda e, kk=kk, bk=bk: e.activation(out=rnf[kk][:], in_=c.ps[bk][:, :], func=AF.Ln, scale=1.0 / 128, bias=eps_ap), reads=[c.psb[bk], c.cb], writes=[rnfb[kk]])
            S.op("act", lambda e, kk=kk: e.activation(out=rnf[kk][:], in_=rnf[kk][:], func=AF.Exp, scale=-0.5), reads=[rnfb[kk]], writes=[rnfb[kk]])
            S.op("dve", lambda e, kk=kk, cs=cs: e.scalar_tensor_tensor(out=rnf[kk][:], in0=odT[:, cs], scalar=c.ppt[:, PP_ON:PP_ON + 1], in1=rnf[kk][:], op0=ALU.mult, op1=ALU.mult), reads=[odTb, rnfb[kk], c.cb], writes=[rnfb[kk]])
            S.op("pool", lambda e, kk=kk, cs=cs: e.tensor_tensor(out=zt[:, cs], in0=rnf[kk][:], in1=zt[:, cs], op=ALU.mult), reads=[rnfb[kk], ztb], writes=[ztb])
        S.dma("sp", c.ydscr[h], zt[:], reads=[ztb], writes=[c.ydb[h]])


def host_layout(inputs):
    f = np.float32
    ev_w_in = inputs["ev_w_in"][0]
    od_w_in = inputs["od_w_in"][0]
    com = {}
    com["w_in0"] = np.ascontiguousarray(ev_w_in[:, :9216].reshape(16, 128, 72, 128).transpose(2, 1, 0, 3))
    com["w_tail0"] = np.ascontiguousarray(ev_w_in[:, 9216:9224].reshape(16, 128, 8).transpose(1, 0, 2))
    com["w_in1"] = np.ascontiguousarray(od_w_in[:, :6144].reshape(16, 128, 48, 128).transpose(2, 1, 0, 3))
    com["w_tail1"] = np.ascontiguousarray(od_w_in[:, 6144:6160].reshape(16, 128, 16).transpose(1, 0, 2))
    com["w_out0"] = np.ascontiguousarray(inputs["ev_w_out"][0].reshape(16, 128, 4, 512).transpose(2, 1, 0, 3))
    com["w_out1"] = np.ascontiguousarray(inputs["od_w_out"][0].reshape(16, 128, 4, 512).transpose(2, 1, 0, 3))
    com["gate_w"] = np.ascontiguousarray(inputs["od_gate_w"][0])
    pp = np.zeros((128, PP_N), f)
    pp[:, PP_EVN:PP_EVN + 16] = inputs["ev_norm"][0].reshape(16, 128).T
    pp[:, PP_ODN:PP_ODN + 16] = inputs["od_norm"][0].reshape(16, 128).T
    pp[:, PP_QN] = inputs["ev_qn_gain"][0]
    pp[:, PP_KN] = inputs["ev_kn_gain"][0]
    pp[:, PP_ON] = inputs["od_onorm"][0]
    ccw = inputs["od_conv_c_w"][0]
    pp[:, PP_CCW:PP_CCW + 32] = ccw.reshape(4, 8, 128).transpose(2, 1, 0).reshape(128, 32)
    pp[:, PP_CCB:PP_CCB + 8] = inputs["od_conv_c_b"][0].reshape(8, 128).T
    gbias = inputs["od_gate_b"][0]
    pp[:, PP_GBR:PP_GBR + 8] = gbias[:1024].reshape(8, 128).T
    pp[:, PP_GBI:PP_GBI + 8] = gbias[1024:].reshape(8, 128).T
    pp[:, PP_LAM:PP_LAM + 8] = inputs["od_lambda"][0].reshape(8, 128).T
    cdw = inputs["od_conv_d_w"][0]
    pp[:, PP_CDW:PP_CDW + 96] = cdw.reshape(4, 24, 128).transpose(2, 1, 0).reshape(128, 96)
    com["pp"] = pp
    hp = np.zeros((8, 4), f)
    hp[0:4, 0] = inputs["ev_if_bias"][0][0:4]
    hp[0:4, 1] = inputs["ev_if_bias"][0][4:8]
    hp[:, 2] = inputs["od_a_log"][0]
    hp[:, 3] = inputs["od_dt_bias"][0]
    com["hp"] = hp
    rb = inputs["ev_rel_bias"][0]
    kl = np.arange(128)[:, None, None]
    jb = np.arange(5)[None, :, None]
    i = np.arange(128)[None, None, :]
    j = jb * 128 + kl
    rel = np.clip(512 + i - j, -256, 256) + 256
    valid = ((i // 64) <= (j // 64)) & ((j // 64) <= 8 + (i // 64))
    tab = rb[:, rel]
    tab = np.where(valid[None], tab, f(NEG)).astype(f)
    com["bt"] = np.ascontiguousarray(tab.reshape(8, 128, 640))
    return com


_CACHE = {}


def kernel(**inputs):
    x = np.ascontiguousarray(inputs["x"], dtype=np.float32)
    com = host_layout(inputs)
    if "nc" not in _CACHE:
        _CACHE["nc"] = build()[0]
    nc = _CACHE["nc"]
    in_maps = []
    for b in range(8):
        m = dict(com)
        m["x"] = x[b]
        in_maps.append(m)
    res = run_bass_kernel_spmd(nc, in_maps, core_ids=list(range(8)))
    out = np.stack([np.asarray(r["out"], dtype=np.float32) for r in res.results], axis=0)
    return out
```
